# Optimizing a Trainium2 kernel written in Bass

```python
import math
import jax, jax.numpy as jnp
from jax import lax
import numpy as np

D_MODEL = 2048
BATCH = 1
SEQ = 16384
DEPTH = 2

N_MIXERS = 2
N_HEADS = 16
HEAD_DIM = D_MODEL // N_HEADS
MOBA_BLOCK = 256
MOBA_TOPK = 3
Q_CHUNK = 64
CONV_WIDTH = 31
N_GROUPS = 4
EXPERTS_PER_GROUP = 8
TOP_K_EXPERTS = 2
D_EXPERT = D_MODEL // 4
LN_EPS = 1e-5
NEG_INF = -1e30
DEEPNORM_ALPHA = (2.0 * DEPTH) ** 0.25
DEEPNORM_BETA = (8.0 * DEPTH) ** -0.25
N_ATTN_LAYERS = (DEPTH + N_MIXERS - 1) // N_MIXERS
N_CONV_LAYERS = DEPTH // N_MIXERS

kernel_name = "hybrid_moba_conformer_hmoe_deepnorm"


def layer_norm(x, g, b):
    xf = x.astype(jnp.float32)
    mu = jnp.mean(xf, axis=-1, keepdims=True)
    var = jnp.mean(jnp.square(xf - mu), axis=-1, keepdims=True)
    y = (xf - mu) * lax.rsqrt(var + LN_EPS) * g.astype(jnp.float32) + b.astype(jnp.float32)
    return y.astype(x.dtype)


def alibi_slopes():
    return 2.0 ** (-8.0 * jnp.arange(1, N_HEADS + 1, dtype=jnp.float32) / N_HEADS)


def moba_attention(x, w_qkv, w_o):
    B, S, D = x.shape
    qkv = x @ w_qkv
    q, k, v = jnp.split(qkv, 3, axis=-1)
    q = q.reshape(B, S, N_HEADS, HEAD_DIM)
    k = k.reshape(B, S, N_HEADS, HEAD_DIM)
    v = v.reshape(B, S, N_HEADS, HEAD_DIM)
    n_blocks = -(-S // MOBA_BLOCK)
    s_pad = n_blocks * MOBA_BLOCK
    k_eff = min(MOBA_TOPK, n_blocks)
    pad = ((0, 0), (0, s_pad - S), (0, 0), (0, 0))
    q_p, k_p, v_p = jnp.pad(q, pad), jnp.pad(k, pad), jnp.pad(v, pad)
    kb = k_p.reshape(B, n_blocks, MOBA_BLOCK, N_HEADS, HEAD_DIM).transpose(0, 3, 1, 2, 4)
    vb = v_p.reshape(B, n_blocks, MOBA_BLOCK, N_HEADS, HEAD_DIM).transpose(0, 3, 1, 2, 4)
    k_mean = jnp.mean(kb.astype(jnp.float32), axis=3)
    slopes = alibi_slopes()
    scale = HEAD_DIM ** -0.5
    blk_ids = jnp.arange(n_blocks)
    b_i = jnp.arange(B)[:, None, None, None]
    h_i = jnp.arange(N_HEADS)[None, None, :, None]
    n_chunks = s_pad // Q_CHUNK

    def chunk_fn(c):
        t0 = c * Q_CHUNK
        own = t0 // MOBA_BLOCK
        t = (t0 + jnp.arange(Q_CHUNK)).astype(jnp.float32)
        qc = lax.dynamic_slice_in_dim(q_p, t0, Q_CHUNK, axis=1)
        gate = jnp.einsum('bqhd,bhnd->bqhn', qc.astype(jnp.float32), k_mean)
        gate = jnp.where((blk_ids < own)[None, None, None, :], gate, NEG_INF)
        _, top_i = lax.top_k(gate, k_eff)
        valid = jnp.arange(k_eff) < own
        kg = kb[b_i, h_i, top_i]
        vg = vb[b_i, h_i, top_i]
        s_sel = jnp.einsum('bqhd,bqhkjd->bqhkj', qc, kg).astype(jnp.float32) * scale
        key_pos = (top_i[..., None] * MOBA_BLOCK + jnp.arange(MOBA_BLOCK)).astype(jnp.float32)
        dist_sel = t[None, :, None, None, None] - key_pos
        s_sel = s_sel - slopes[None, None, :, None, None] * dist_sel
        s_sel = jnp.where(valid[None, None, None, :, None], s_sel, NEG_INF)
        ko = lax.dynamic_slice_in_dim(k_p, own * MOBA_BLOCK, MOBA_BLOCK, axis=1)
        vo = lax.dynamic_slice_in_dim(v_p, own * MOBA_BLOCK, MOBA_BLOCK, axis=1)
        s_own = jnp.einsum('bqhd,bkhd->bqhk', qc, ko).astype(jnp.float32) * scale
        own_pos = (own * MOBA_BLOCK + jnp.arange(MOBA_BLOCK)).astype(jnp.float32)
        dist_own = t[:, None] - own_pos[None, :]
        s_own = s_own - slopes[None, None, :, None] * dist_own[None, :, None, :]
        s_own = jnp.where((dist_own >= 0)[None, :, None, :], s_own, NEG_INF)
        scores = jnp.concatenate(
            [s_sel.reshape(B, Q_CHUNK, N_HEADS, k_eff * MOBA_BLOCK), s_own], axis=-1)
        p = jax.nn.softmax(scores, axis=-1).astype(v.dtype)
        p_sel = p[..., :k_eff * MOBA_BLOCK].reshape(B, Q_CHUNK, N_HEADS, k_eff, MOBA_BLOCK)
        p_own = p[..., k_eff * MOBA_BLOCK:]
        return (jnp.einsum('bqhkj,bqhkjd->bqhd', p_sel, vg)
                + jnp.einsum('bqhk,bkhd->bqhd', p_own, vo))

    outs = lax.map(chunk_fn, jnp.arange(n_chunks))
    o = jnp.moveaxis(outs, 0, 1).reshape(B, s_pad, D)[:, :S]
    return o @ w_o


def conformer_conv(x, w_pw1, b_pw1, w_dw, b_dw, ln_g, ln_b, w_pw2):
    D = x.shape[-1]
    h = x @ w_pw1 + b_pw1
    h = h[..., :D] * jax.nn.sigmoid(h[..., D:])
    h = lax.conv_general_dilated(
        h, w_dw.reshape(CONV_WIDTH, 1, D),
        window_strides=(1,), padding=[(CONV_WIDTH - 1, 0)],
        dimension_numbers=('NWC', 'WIO', 'NWC'),
        feature_group_count=D) + b_dw
    h = jax.nn.silu(layer_norm(h, ln_g, ln_b))
    return h @ w_pw2


def hierarchical_moe(x, w_grp, b_grp, w_rt, b_rt, w_gate, w_up, w_down):
    B, S, D = x.shape
    xt = x.reshape(B * S, D)
    g_logits = (xt @ w_grp + b_grp).astype(jnp.float32)
    g_prob = jax.nn.softmax(g_logits, axis=-1)
    g_sel = jnp.argmax(g_logits, axis=-1)
    g_p = jnp.take_along_axis(g_prob, g_sel[:, None], axis=1)
    e_logits = (jnp.einsum('td,dge->tge', xt, w_rt) + b_rt).astype(jnp.float32)
    e_logits = jnp.take_along_axis(e_logits, g_sel[:, None, None], axis=1)[:, 0]
    top_v, top_i = lax.top_k(e_logits, TOP_K_EXPERTS)
    w2 = jax.nn.softmax(top_v, axis=-1) * g_p
    onehot_g = jax.nn.one_hot(g_sel, N_GROUPS, dtype=jnp.float32)
    e_gate = jnp.einsum('tk,tke->te', w2, jax.nn.one_hot(top_i, EXPERTS_PER_GROUP, dtype=jnp.float32))
    comb = (onehot_g[:, :, None] * e_gate[:, None, :]).astype(x.dtype)
    y = jnp.zeros_like(xt)
    for g in range(N_GROUPS):
        h = jax.nn.silu(jnp.einsum('td,edf->tef', xt, w_gate[g])) * jnp.einsum('td,edf->tef', xt, w_up[g])
        h = h * comb[:, g, :, None]
        y = y + jnp.einsum('tef,efd->td', h, w_down[g])
    return y.reshape(B, S, D)


def setup_inputs(seed: int = 0) -> dict:
    key = jax.random.key(seed)
    ks = jax.random.split(key, 24)
    D, F, G, E = D_MODEL, D_EXPERT, N_GROUPS, EXPERTS_PER_GROUP
    f32 = jnp.float32
    nrm = lambda k, shape, s: jax.random.normal(k, shape, f32) * s
    ones_n = lambda k, shape: 1.0 + 0.02 * jax.random.normal(k, shape, f32)
    qkv_scale = jnp.concatenate([jnp.ones((2 * D,), f32), jnp.full((D,), DEEPNORM_BETA, f32)])
    return {
        "x": jax.random.normal(ks[0], (BATCH, SEQ, D), f32),
        "attn_w_qkv": nrm(ks[1], (N_ATTN_LAYERS, D, 3 * D), D ** -0.5) * qkv_scale,
        "attn_w_o": nrm(ks[2], (N_ATTN_LAYERS, D, D), DEEPNORM_BETA * D ** -0.5),
        "conv_w_pw1": nrm(ks[3], (N_CONV_LAYERS, D, 2 * D), D ** -0.5),
        "conv_b_pw1": nrm(ks[4], (N_CONV_LAYERS, 2 * D), 0.02),
        "conv_w_dw": nrm(ks[5], (N_CONV_LAYERS, CONV_WIDTH, D), CONV_WIDTH ** -0.5),
        "conv_b_dw": nrm(ks[6], (N_CONV_LAYERS, D), 0.02),
        "conv_ln_g": ones_n(ks[7], (N_CONV_LAYERS, D)),
        "conv_ln_b": nrm(ks[8], (N_CONV_LAYERS, D), 0.02),
        "conv_w_pw2": nrm(ks[9], (N_CONV_LAYERS, D, D), DEEPNORM_BETA * D ** -0.5),
        "mix_ln_g": ones_n(ks[10], (DEPTH, D)),
        "mix_ln_b": nrm(ks[11], (DEPTH, D), 0.02),
        "moe_w_grp": nrm(ks[12], (DEPTH, D, G), D ** -0.5),
        "moe_b_grp": nrm(ks[13], (DEPTH, G), 0.01),
        "moe_w_rt": nrm(ks[14], (DEPTH, D, G, E), D ** -0.5),
        "moe_b_rt": nrm(ks[15], (DEPTH, G, E), 0.01),
        "moe_w_gate": nrm(ks[16], (DEPTH, G, E, D, F), D ** -0.5),
        "moe_w_up": nrm(ks[17], (DEPTH, G, E, D, F), D ** -0.5),
        "moe_w_down": nrm(ks[18], (DEPTH, G, E, F, D), DEEPNORM_BETA * F ** -0.5),
        "ffn_ln_g": ones_n(ks[19], (DEPTH, D)),
        "ffn_ln_b": nrm(ks[20], (DEPTH, D), 0.02),
    }


def reference(x, attn_w_qkv, attn_w_o, conv_w_pw1, conv_b_pw1, conv_w_dw, conv_b_dw,
              conv_ln_g, conv_ln_b, conv_w_pw2, mix_ln_g, mix_ln_b, moe_w_grp, moe_b_grp,
              moe_w_rt, moe_b_rt, moe_w_gate, moe_w_up, moe_w_down, ffn_ln_g, ffn_ln_b):
    for i in range(DEPTH):
        j = i // N_MIXERS
        if i % N_MIXERS == 0:
            y = moba_attention(x, attn_w_qkv[j], attn_w_o[j])
        else:
            y = conformer_conv(x, conv_w_pw1[j], conv_b_pw1[j], conv_w_dw[j], conv_b_dw[j],
                               conv_ln_g[j], conv_ln_b[j], conv_w_pw2[j])
        x = layer_norm(DEEPNORM_ALPHA * x + y, mix_ln_g[i], mix_ln_b[i])
        y = hierarchical_moe(x, moe_w_grp[i], moe_b_grp[i], moe_w_rt[i], moe_b_rt[i],
                             moe_w_gate[i], moe_w_up[i], moe_w_down[i])
        x = layer_norm(DEEPNORM_ALPHA * x + y, ffn_ln_g[i], ffn_ln_b[i])
    return x
```

```python
from contextlib import ExitStack
import numpy as np
import concourse.bass as bass
import concourse.mybir as mybir
from concourse.bass_utils import run_bass_kernel_spmd


F32 = mybir.dt.float32
BF16 = mybir.dt.bfloat16
I32 = mybir.dt.int32
AF = mybir.ActivationFunctionType
ALU = mybir.AluOpType
AX = mybir.AxisListType

ENGS = ["tensor", "vector", "scalar", "gpsimd", "sync"]
EPOCH = 3000
N_DMA_SEMS = 36
DMA_POOLS = {"sync": (0, 12), "gpsimd": (12, 24), "scalar": (24, 36), "vector": (24, 36), "tensor": (24, 36)}


class Op:
    __slots__ = ("eng", "fn", "reads", "writes", "dma", "deps", "needs_inc",
                 "inc_sem", "inc_val", "idx", "dma_slot")

    def __init__(self, eng, fn, reads, writes, dma):
        self.eng = eng
        self.fn = fn
        self.reads = tuple(reads)
        self.writes = tuple(writes)
        self.dma = dma
        self.deps = []
        self.needs_inc = False
        self.inc_sem = None
        self.inc_val = None
        self.dma_slot = None


class Prog:
    def __init__(self, nc, same_engine_sync=True):
        self.nc = nc
        self.ops = []
        self.same_engine_sync = same_engine_sync
        self.final_waits = []

    @staticmethod
    def is_psum(k):
        if isinstance(k, tuple):
            k = k[0]
        return isinstance(k, str) and k.startswith("ps_")

    def op(self, eng, fn, reads=(), writes=(), dma=False):
        o = Op(eng, fn, reads, writes, dma)
        o.idx = len(self.ops)
        self.ops.append(o)
        return o

    def pe(self, fn, reads=(), writes=()):
        return self.op("tensor", fn, reads, writes)

    def dve(self, fn, reads=(), writes=()):
        return self.op("vector", fn, reads, writes)

    def act(self, fn, reads=(), writes=()):
        return self.op("scalar", fn, reads, writes)

    def pool(self, fn, reads=(), writes=()):
        return self.op("gpsimd", fn, reads, writes)

    def dma(self, eng, fn, reads=(), writes=()):
        return self.op(eng, fn, reads, writes, dma=True)

    def finish(self, out_ops):
        self.final_waits = list(out_ops)

    def build(self, stack, semstack=None, prefix=""):
        if semstack is None:
            semstack = stack
        nc = self.nc
        last_writer = {}
        readers = {}
        dma_rr = {e: DMA_POOLS[e][0] for e in ENGS}
        dma_last = [None] * N_DMA_SEMS
        for o in self.ops:
            deps = set()
            for r in o.reads:
                w = last_writer.get(r)
                if w is not None:
                    deps.add(w)
                if self.is_psum(r):
                    for rd in readers.get(r, ()):
                        if rd.eng != o.eng:
                            deps.add(rd)
            for wk in o.writes:
                w = last_writer.get(wk)
                if w is not None:
                    deps.add(w)
                for rd in readers.get(wk, ()):
                    deps.add(rd)
            deps.discard(o)
            if o.dma:
                slot = dma_rr[o.eng]
                o.dma_slot = slot
                prev = dma_last[slot]
                if prev is not None:
                    deps.add(prev)
                dma_last[slot] = o
                lo, hi = DMA_POOLS[o.eng]
                dma_rr[o.eng] = lo + (slot + 1 - lo) % (hi - lo)
            final = []
            for d in deps:
                if (not d.dma) and d.eng == o.eng and not o.dma:
                    if o.eng == "tensor" or not self.same_engine_sync:
                        continue
                if (not d.dma) and d.eng == o.eng and o.dma and o.eng == "tensor":
                    continue
                final.append(d)
                d.needs_inc = True
            o.deps = final
            for r in o.reads:
                readers.setdefault(r, []).append(o)
            for wk in o.writes:
                last_writer[wk] = o
                readers[wk] = []
        for d in self.final_waits:
            d.needs_inc = True
        cnt = {e: 0 for e in ENGS}
        dma_cnt = [0] * N_DMA_SEMS
        n_epochs = {e: 1 for e in ENGS}
        for o in self.ops:
            if o.dma:
                dma_cnt[o.dma_slot] += 1
                o.inc_sem = ("dma", o.dma_slot)
                o.inc_val = 16 * dma_cnt[o.dma_slot]
            elif o.needs_inc:
                cnt[o.eng] += 1
                ep = (cnt[o.eng] - 1) // EPOCH
                o.inc_sem = (o.eng, ep)
                o.inc_val = cnt[o.eng] - ep * EPOCH
                n_epochs[o.eng] = max(n_epochs[o.eng], ep + 1)
        sems = {}
        for e in ENGS:
            for ep in range(n_epochs[e]):
                sems[(e, ep)] = semstack.enter_context(nc.semaphore(f"{prefix}c_{e}_{ep}"))
        for i in range(N_DMA_SEMS):
            sems[("dma", i)] = semstack.enter_context(nc.semaphore(f"{prefix}d_{i}"))
        block = stack.enter_context(nc.Block())
        by_eng = {e: [o for o in self.ops if o.eng == e] for e in ENGS}
        final_waits = self.final_waits
        stats = {e: [0, 0] for e in ENGS}

        def emit_engine(e, eng):
            waited = {}
            for o in by_eng[e]:
                for d in o.deps:
                    k = d.inc_sem
                    if waited.get(k, 0) >= d.inc_val:
                        continue
                    eng.wait_ge(sems[k], d.inc_val)
                    waited[k] = d.inc_val
                    stats[e][1] += 1
                ins = o.fn(eng)
                stats[e][0] += 1
                if o.dma:
                    ins.then_inc(sems[o.inc_sem], 16)
                elif o.needs_inc:
                    ins.then_inc(sems[o.inc_sem], 1)
            if e == "sync":
                for i in range(N_DMA_SEMS):
                    if dma_cnt[i] > 0 and waited.get(("dma", i), 0) < 16 * dma_cnt[i]:
                        eng.wait_ge(sems[("dma", i)], 16 * dma_cnt[i])
                        waited[("dma", i)] = 16 * dma_cnt[i]
                for d in final_waits:
                    k = d.inc_sem
                    if waited.get(k, 0) >= d.inc_val:
                        continue
                    eng.wait_ge(sems[k], d.inc_val)
                    waited[k] = d.inc_val

        @block.tensor
        def _(eng):
            emit_engine("tensor", eng)

        @block.vector
        def _(eng):
            emit_engine("vector", eng)

        @block.scalar
        def _(eng):
            emit_engine("scalar", eng)

        @block.gpsimd
        def _(eng):
            emit_engine("gpsimd", eng)

        @block.sync
        def _(eng):
            emit_engine("sync", eng)

        self.stats = stats
        return stats


NBT = 272
ATT_SCALE = 128.0 ** -0.5


def attn_consts(core):
    bt = np.zeros((2, 128, NBT), np.float32)
    fc = np.zeros((2, 128, 4), np.float32)
    p = np.arange(128, dtype=np.float64)[:, None]
    m = (np.arange(NBT, dtype=np.float64) - 8.0)[None, :]
    for j in range(2):
        h = 2 * core + j
        slope = 2.0 ** (-8.0 * (h + 1) / 16.0)
        bt[j] = (slope * (p - 64.0 * m)).astype(np.float32)
        for t in range(4):
            fc[j, :, t] = np.float32(np.exp(-slope * (320.0 + 128.0 * t)))
    return bt, fc


def build_attn(NG):
    S = NG * 512
    nc = bass.Bass("TRN2", target_bir_lowering=False)
    x_d = nc.dram_tensor("x", [S, 2048], F32, kind="ExternalInput").ap()
    w_d = nc.dram_tensor("wqkv", [2, 2048, 384], F32, kind="ExternalInput").ap()
    bt_d = nc.dram_tensor("btab", [2, 128, NBT], F32, kind="ExternalInput").ap()
    fc_d = nc.dram_tensor("fcorr", [2, 128, 4], F32, kind="ExternalInput").ap()
    o_d = nc.dram_tensor("o", [S, 256], F32, kind="ExternalOutput").ap()
    NT = S // 128
    with ExitStack() as st:
        def sb(name, shape, dt):
            return st.enter_context(nc.sbuf_tensor(name, shape, dt))

        def ps(name, shape, dt):
            return st.enter_context(nc.psum_tensor(name, shape, dt))

        w_sb = sb("w_sb", [128, 16, 384], BF16)
        xb = [sb(f"xb{i}", [128, 2048], BF16) for i in range(8)]
        xT = sb("xT", [128, 16, 512], BF16)
        qT = [sb(f"qT{i}", [128, 512], BF16) for i in range(2)]
        qf = sb("qf", [128, 512], F32)
        kT = sb("kT", [128, S], BF16)
        Vp = sb("Vp", [128, NT, 129], BF16)
        ksum = sb("ksum", [128, 64], F32)
        gate = sb("gate", [128, 4, 64], F32)
        mx = sb("mx", [128, 4, 8], F32)
        W = [sb(f"W{i}", [128, 4, 64], F32) for i in range(2)]
        NPT = 4
        pT = [sb(f"pT{i}", [128, 512], BF16) for i in range(NPT)]
        Oacc = [sb(f"Oacc{i}", [128, 4, 129], F32) for i in range(2)]
        rden = sb("rden", [128, 4], F32)
        ost = [sb(f"ost{i}", [128, 4, 128], F32) for i in range(2)]
        identf = sb("identf", [128, 128], F32)
        ident = sb("ident", [128, 128], BF16)
        trif = sb("trif", [128, 128], F32)
        tri = sb("tri", [128, 128], BF16)
        btab = sb("btab_sb", [128, NBT], F32)
        fcorr = sb("fcorr_sb", [128, 4], F32)

        NS = 4
        s_ps = [ps(f"s{i}", [128, 512], F32) for i in range(NS)]
        po = [[ps(f"po{i}{j}", [128, 512], F32) for j in range(2)] for i in range(2)]
        tr = [s_ps[i][:].bitcast(BF16).rearrange("p (a b) -> p a b", b=128) for i in range(2)]
        pq, pk, pv, pg = po[0][0], po[0][1], po[1][0], po[1][1]
        K_pq, K_pk, K_pv, K_pg = "ps_po00", "ps_po01", "ps_po10", "ps_po11"

        P = Prog(nc)
        P.pool(lambda e: e.memset(identf[:], 1.0), writes=["identf"])
        P.pool(lambda e: e.affine_select(out=identf[:], in_=identf[:], pattern=[[-1, 128]],
                                         compare_op=ALU.is_equal, fill=0.0, base=0,
                                         channel_multiplier=1), reads=["identf"], writes=["identf"])
        P.dve(lambda e: e.tensor_copy(out=ident[:], in_=identf[:]), reads=["identf"], writes=["ident"])
        P.pool(lambda e: e.memset(trif[:], 1.0), writes=["trif"])
        P.pool(lambda e: e.affine_select(out=trif[:], in_=trif[:], pattern=[[1, 128]],
                                         compare_op=ALU.is_ge, fill=0.0, base=0,
                                         channel_multiplier=-1), reads=["trif"], writes=["trif"])
        P.dve(lambda e: e.tensor_copy(out=tri[:], in_=trif[:]), reads=["trif"], writes=["tri"])
        P.dve(lambda e: e.memset(ksum[:], 0.0), writes=[("ksum", gg) for gg in range(NG)])
        P.dve(lambda e: e.memset(Vp[:, :, 128:129], 1.0), writes=["Vones"])

        out_ops = []
        pt_rr = [0]
        sq_rr = [0]
        bank_cnt = {}
        bank_tot = {}
        blk_par = [0]

        for hh in range(2):
            P.dve(lambda e: e.memset(gate[:], -1e30), writes=[("gate", t) for t in range(4)])
            P.dma("gpsimd", lambda e, hh=hh: e.dma_start(
                out=w_sb[:], in_=w_d[hh].rearrange("(c p) n -> p c n", p=128)), writes=["w"])
            P.dma("sync", lambda e, hh=hh: e.dma_start(out=btab[:], in_=bt_d[hh]), writes=["btab"])
            P.dma("sync", lambda e, hh=hh: e.dma_start(out=fcorr[:], in_=fc_d[hh]), writes=["fcorr"])
            for g in range(NG):
                gp = g % 2
                for i in range(4):
                    bi = gp * 4 + i
                    tok0 = g * 512 + i * 128
                    P.dma("gpsimd", lambda e, bi=bi, tok0=tok0: e.dma_start(
                        out=xb[bi][:], in_=x_d[tok0:tok0 + 128, :]), writes=[("xb", bi)])
                for i in range(4):
                    bi = gp * 4 + i
                    for half in range(2):
                        tb = half
                        for cc in range(8):
                            c = half * 8 + cc
                            P.pe(lambda e, bi=bi, c=c, tb=tb, cc=cc: e.transpose(
                                out=tr[tb][:, cc, :], in_=xb[bi][:, c * 128:(c + 1) * 128], identity=ident[:]),
                                reads=[("xb", bi), "ident"], writes=[("ps_s", tb)])
                        if half == 0:
                            P.act(lambda e, tb=tb, half=half, i=i: e.copy(
                                out=xT[:, half * 8:(half + 1) * 8, i * 128:(i + 1) * 128], in_=tr[tb]),
                                reads=[("ps_s", tb)], writes=[("xT", i, half)])
                        else:
                            P.dve(lambda e, tb=tb, half=half, i=i: e.tensor_copy(
                                out=xT[:, half * 8:(half + 1) * 8, i * 128:(i + 1) * 128], in_=tr[tb]),
                                reads=[("ps_s", tb)], writes=[("xT", i, half)])
                xT_keys = [("xT", i, half) for i in range(4) for half in range(2)]
                for c in range(16):
                    P.pe(lambda e, c=c: e.matmul(pq[:], lhsT=w_sb[:, c, 0:128], rhs=xT[:, c, :],
                                                 start=(c == 0), stop=(c == 15)),
                         reads=["w"] + xT_keys, writes=[K_pq])
                for c in range(16):
                    P.pe(lambda e, c=c: e.matmul(pk[:], lhsT=w_sb[:, c, 128:256], rhs=xT[:, c, :],
                                                 start=(c == 0), stop=(c == 15)),
                         reads=["w"] + xT_keys, writes=[K_pk])
                for i in range(4):
                    for c in range(16):
                        P.pe(lambda e, c=c, i=i: e.matmul(pv[:, i * 128:(i + 1) * 128],
                                                          lhsT=xT[:, c, i * 128:(i + 1) * 128],
                                                          rhs=w_sb[:, c, 256:384],
                                                          start=(c == 0), stop=(c == 15)),
                             reads=["w"] + xT_keys, writes=[K_pv])
                P.act(lambda e, gp=gp: e.copy(out=qT[gp][:], in_=pq[:]), reads=[K_pq], writes=[("qT", gp)])
                P.dve(lambda e: e.tensor_copy(out=qf[:], in_=pq[:]), reads=[K_pq], writes=["qf"])
                P.act(lambda e, g=g: e.copy(out=kT[:, g * 512:(g + 1) * 512], in_=pk[:]),
                      reads=[K_pk], writes=[("kT", g)])
                P.dve(lambda e, g=g: e.tensor_reduce(
                    out=ksum[:, 2 * g:2 * g + 2], in_=pk[:].rearrange("p (a b) -> p a b", b=256),
                    axis=AX.X, op=ALU.add), reads=[K_pk], writes=[("ksum", g)])
                P.dve(lambda e, g=g: e.tensor_copy(
                    out=Vp[:, 4 * g:4 * g + 4, 0:128], in_=pv[:].rearrange("p (a b) -> p a b", b=128)),
                    reads=[K_pv], writes=[("V", g)])
                ksum_keys = [("ksum", gg) for gg in range(g + 1)]
                for t in range(4):
                    own = 2 * g + t // 2
                    if own == 0:
                        continue
                    P.pe(lambda e, t=t: e.matmul(pg[:, t * 64:(t + 1) * 64], lhsT=qf[:, t * 128:(t + 1) * 128],
                                                 rhs=ksum[:, 0:64], start=True, stop=True),
                         reads=["qf"] + ksum_keys, writes=[K_pg])
                for t in range(4):
                    own = 2 * g + t // 2
                    if own == 0:
                        continue
                    P.dve(lambda e, t=t, own=own: e.tensor_copy(out=gate[:, t, 0:own], in_=pg[:, t * 64:t * 64 + own]),
                          reads=[K_pg], writes=[("gate", t)])
                    P.dve(lambda e, t=t: e.max(out=mx[:, t, :], in_=gate[:, t, :]),
                          reads=[("gate", t)], writes=[("mx", t)])
                    P.dve(lambda e, t=t, gp=gp: e.tensor_scalar(
                        out=W[gp][:, t, :], in0=gate[:, t, :], scalar1=mx[:, t, 2:3], scalar2=None, op0=ALU.is_ge),
                        reads=[("gate", t), ("mx", t)], writes=[("W", gp, t)])
                    nfar = 2 * g - 1
                    if nfar > 0:
                        P.dve(lambda e, t=t, gp=gp, nfar=nfar: e.tensor_scalar(
                            out=W[gp][:, t, 0:nfar], in0=W[gp][:, t, 0:nfar], scalar1=fcorr[:, t:t + 1],
                            scalar2=None, op0=ALU.mult),
                            reads=[("W", gp, t), "fcorr"], writes=[("W", gp, t)])

                jobs = []

                def do_tile(kt, acts, pv_list, masked_sub=None):
                    rec = {"si": None}
                    cmin = min(a[0] for a in acts)

                    def emit_qk(kt=kt, cmin=cmin, rec=rec, gp=gp):
                        si = sq_rr[0] % NS
                        sq_rr[0] += 1
                        rec["si"] = si
                        P.pe(lambda e, kt=kt, si=si, cmin=cmin, gp=gp: e.matmul(
                            s_ps[si][:, cmin:512], lhsT=kT[:, kt * 128:(kt + 1) * 128], rhs=qT[gp][:, cmin:512],
                            start=True, stop=True),
                            reads=[("kT", kt // 4), ("qT", gp)], writes=[("ps_s", si)])

                    def emit_rest(kt=kt, acts=acts, pv_list=pv_list, masked_sub=masked_sub, rec=rec):
                        si = rec["si"]
                        pi = pt_rr[0] % NPT
                        pt_rr[0] += 1
                        for (c0, c1, m) in acts:
                            P.act(lambda e, si=si, pi=pi, c0=c0, c1=c1, m=m: e.activation(
                                out=pT[pi][:, c0:c1], in_=s_ps[si][:, c0:c1], func=AF.Exp,
                                bias=btab[:, m + 8:m + 9], scale=ATT_SCALE),
                                reads=[("ps_s", si), "btab"],
                                writes=[("pT", pi, ss) for ss in range(c0 // 128, c1 // 128)])
                        if masked_sub is not None:
                            c0 = masked_sub * 128
                            P.pool(lambda e, pi=pi, c0=c0: e.tensor_tensor(
                                out=pT[pi][:, c0:c0 + 128], in0=pT[pi][:, c0:c0 + 128], in1=tri[:], op=ALU.mult),
                                reads=[("pT", pi, masked_sub), "tri"], writes=[("pT", pi, masked_sub)])
                        for (t, po_t, pkey, stt, stp) in pv_list:
                            cnt_ = bank_cnt[pkey]
                            bank_cnt[pkey] += 1
                            stt = (cnt_ == 0)
                            stp = (cnt_ == bank_tot[pkey] - 1)
                            P.pe(lambda e, t=t, po_t=po_t, pi=pi, kt=kt, stt=stt, stp=stp: e.matmul(
                                po_t[:, (t % 2) * 129:(t % 2) * 129 + 129], lhsT=pT[pi][:, t * 128:(t + 1) * 128],
                                rhs=Vp[:, kt, :], start=stt, stop=stp),
                                reads=[("pT", pi, t), ("V", kt // 4), "Vones"], writes=[pkey])
                    jobs.append(("tile", emit_qk, emit_rest))

                def po_for(t, bp):
                    return po[bp][t // 2], f"ps_po{bp}{t // 2}"

                def new_round(bp, n0=4, n1=4):
                    def f(bp=bp, n0=n0, n1=n1):
                        bank_cnt[f"ps_po{bp}0"] = 0
                        bank_cnt[f"ps_po{bp}1"] = 0
                        bank_tot[f"ps_po{bp}0"] = n0
                        bank_tot[f"ps_po{bp}1"] = n1
                    jobs.append(("call", f))

                def accum(t, bp, mode, wcol=None):
                    jobs.append(("call", lambda t=t, bp=bp, mode=mode, wcol=wcol: accum_now(t, bp, mode, wcol)))

                def accum_now(t, bp, mode, wcol=None):
                    po_t, pkey = po_for(t, bp)
                    src = po_t[:, (t % 2) * 129:(t % 2) * 129 + 129]
                    if mode == "init":
                        P.dve(lambda e, t=t, src=src, gp=gp: e.tensor_copy(out=Oacc[gp][:, t, :], in_=src),
                              reads=[pkey], writes=[("Oacc", gp, t)])
                    else:
                        P.dve(lambda e, t=t, src=src, wcol=wcol, gp=gp: e.scalar_tensor_tensor(
                            out=Oacc[gp][:, t, :], in0=src, scalar=W[gp][:, t, wcol:wcol + 1],
                            in1=Oacc[gp][:, t, :], op0=ALU.mult, op1=ALU.add),
                            reads=[pkey, ("W", gp, t), ("Oacc", gp, t)], writes=[("Oacc", gp, t)])

                k0 = 4 * g
                bp = blk_par[0] % 2
                blk_par[0] += 1
                new_round(bp, 0, 3)
                p2, k2 = po_for(2, bp)
                do_tile(k0 + 2, [(256, 384, 1), (384, 512, 3)],
                        [(2, p2, k2, True, True), (3, p2, k2, True, False)], masked_sub=2)
                do_tile(k0 + 3, [(384, 512, 1)], [(3, p2, k2, False, True)], masked_sub=3)
                accum(2, bp, "init")
                accum(3, bp, "init")
                bp = blk_par[0] % 2
                blk_par[0] += 1
                new_round(bp, 3, 4)
                pa, ka = po_for(0, bp)
                pb_, kb_ = po_for(2, bp)
                do_tile(k0 + 0, [(0, 128, 1), (128, 256, 3), (256, 384, 5), (384, 512, 7)],
                        [(0, pa, ka, True, True), (1, pa, ka, True, False),
                         (2, pb_, kb_, True, False), (3, pb_, kb_, True, False)], masked_sub=0)
                do_tile(k0 + 1, [(128, 256, 1), (256, 384, 3), (384, 512, 5)],
                        [(1, pa, ka, False, True), (2, pb_, kb_, False, True), (3, pb_, kb_, False, True)],
                        masked_sub=1)
                accum(0, bp, "init")
                accum(1, bp, "init")
                accum(2, bp, "acc", wcol=2 * g)
                accum(3, bp, "acc", wcol=2 * g)
                if g >= 1:
                    n = 2 * g - 1
                    bp = blk_par[0] % 2
                    blk_par[0] += 1
                    new_round(bp)
                    for jj in range(2):
                        kt = 2 * n + jj
                        rel = kt - k0
                        acts = [(s * 128, (s + 1) * 128, 2 * (s - rel) + 1) for s in range(4)]
                        pvl = []
                        for t in range(4):
                            p_t, k_t = po_for(t, bp)
                            pvl.append((t, p_t, k_t, jj == 0, jj == 1))
                        do_tile(kt, acts, pvl)
                    for t in range(4):
                        accum(t, bp, "acc", wcol=n)
                for n in range(0, 2 * g - 1):
                    bp = blk_par[0] % 2
                    blk_par[0] += 1
                    new_round(bp)
                    for jj in range(2):
                        kt = 2 * n + jj
                        m = 2 * (k0 - 2 - kt)
                        pvl = []
                        for t in range(4):
                            p_t, k_t = po_for(t, bp)
                            pvl.append((t, p_t, k_t, jj == 0, jj == 1))
                        do_tile(kt, [(0, 512, m)], pvl)
                    for t in range(4):
                        accum(t, bp, "acc", wcol=n)
                LOOK = NS - 1
                tile_idx = [i for i, j in enumerate(jobs) if j[0] == "tile"]
                qk_done = 0
                for pos, job in enumerate(jobs):
                    if job[0] == "call":
                        job[1]()
                        continue
                    my = tile_idx.index(pos)
                    while qk_done < len(tile_idx) and qk_done <= my + LOOK:
                        jobs[tile_idx[qk_done]][1]()
                        qk_done += 1
                    job[2]()
                oacc_keys = [("Oacc", gp, t) for t in range(4)]
                P.dve(lambda e, gp=gp: e.reciprocal(out=rden[:], in_=Oacc[gp][:, :, 128]),
                      reads=oacc_keys, writes=["rden"])
                for t in range(4):
                    P.dve(lambda e, gp=gp, t=t: e.tensor_scalar(
                        out=ost[gp][:, t, :], in0=Oacc[gp][:, t, 0:128], scalar1=rden[:, t:t + 1],
                        scalar2=None, op0=ALU.mult),
                        reads=[("Oacc", gp, t), "rden"], writes=[("ost", gp)])
                oo = P.dma("sync", lambda e, gp=gp, g=g, hh=hh: e.dma_start(
                    out=o_d[g * 512:(g + 1) * 512, hh * 128:(hh + 1) * 128].rearrange("(t p) d -> p t d", p=128),
                    in_=ost[gp][:]), reads=[("ost", gp)])
                out_ops.append(oo)
        P.finish(out_ops)
        stats = P.build(st)
        print("attn stats", stats)
    return nc


ALPHA = float((2.0 * 2) ** 0.25)
LN_EPS = 1e-5
CAP = 256
NSLOT = 32 * CAP
BIGIDX = 1.0e6


def build_rest(NTL=17, NEXP=32, do_layer1=True):
    NMAIN = NTL - 1
    NQ = NMAIN // 4
    nc = bass.Bass("TRN2", target_bir_lowering=False)

    def din(name, shape, dt=F32):
        return nc.dram_tensor(name, shape, dt, kind="ExternalInput").ap()

    xin = din("xin", [NTL * 128, 2048])
    ain = din("ain", [NTL * 128, 2048])
    wo_d = din("wo", [2048, 2048])
    lng_d = din("lng", [4, 128, 2048])
    lnb_d = din("lnb", [4, 128, 2048])
    wr_d = din("wr", [2, 2048, 36])
    br_d = din("br", [2, 128, 36])
    wg_d = din("wgate", [2, 32, 2048, 512])
    wu_d = din("wup", [2, 32, 2048, 512])
    wd_d = din("wdown", [2, 32, 512, 2048])
    wpw1_d = din("wpw1", [2048, 4096])
    bpw1_d = din("bpw1", [128, 32])
    wdw_d = din("wdw", [128, 16, 31])
    bdw_d = din("bdw", [128, 16])
    cg_d = din("cg", [128, 16])
    cb_d = din("cb", [128, 16])
    wpw2_d = din("wpw2", [2048, 2048])
    halo_d = din("halo", [128, 1])
    ebase_d = din("ebase", [128, 32])
    out_d = nc.dram_tensor("out", [NMAIN * 128, 2048], F32, kind="ExternalOutput").ap()
    XM = nc.dram_tensor("XM", [2, NTL * 128, 2048], F32, kind="Internal").ap()
    X1 = nc.dram_tensor("X1", [NTL * 128, 2048], F32, kind="Internal").ap()
    Xd = nc.dram_tensor("Xd", [NSLOT, 2048], BF16, kind="Internal").ap()
    Yd = nc.dram_tensor("Yd", [NSLOT, 2048], F32, kind="Internal").ap()

    with ExitStack() as st:
        def sb(name, shape, dt):
            return st.enter_context(nc.sbuf_tensor(name, shape, dt))

        def ps(name, shape, dt):
            return st.enter_context(nc.psum_tensor(name, shape, dt))

        BIG = sb("BIG", [128, 49152], BF16)
        G8 = [sb(f"G8_{i}", [128, 2048], F32) for i in range(6)]
        B4 = [sb(f"B4_{i}", [128, 2048], BF16) for i in range(4)]
        XET = sb("XET", [128, 16, 256], BF16)
        XETf = XET[:].rearrange("p c t -> p (c t)")
        T16 = XETf[:, 0:2048].rearrange("p (c t) -> p c t", t=128)
        HT = sb("HT", [128, 4, 256], BF16)
        identf = sb("identf", [128, 128], F32)
        ident = sb("ident", [128, 128], BF16)
        Umat = sb("Umat", [128, 128], F32)
        ONESf = sb("ONESf", [128, 128], F32)
        wr_sb = sb("wr_sb", [128, 16, 36], F32)
        br_sb = sb("br_sb", [128, 36], F32)
        ebase = sb("ebase_sb", [128, 32], F32)
        halo = sb("halo_sb", [128, 1], F32)
        bpw1 = sb("bpw1_sb", [128, 32], F32)
        wdw = sb("wdw_sb", [128, 16, 31], F32)
        bdw = sb("bdw_sb", [128, 16], F32)
        cg = sb("cg_sb", [128, 16], F32)
        cb = sb("cb_sb", [128, 16], F32)
        idx1a = sb("idx1", [128, 2, NTL], I32)
        idx2a = sb("idx2", [128, 2, NTL], I32)
        wt1a = sb("wt1", [128, 2, NTL], F32)
        wt2a = sb("wt2", [128, 2, NTL], F32)
        indcum = sb("indcum", [128, 32], F32)
        rs = sb("rs", [128, 16], F32)
        slotv = sb("slotv", [128, 32], F32)
        valid = sb("valid", [128, 32], F32)
        tmp32 = sb("tmp32", [128, 32], F32)
        idxf = sb("idxf", [128, 2], F32)
        stt6 = sb("stt6", [128, 4, 6], F32)
        mv = sb("mv", [128, 2], F32)
        lnsc = sb("lnsc", [128, 2], F32)
        epsv = sb("epsv", [128, 1], F32)

        TR = [ps(f"TR{i}", [128, 8, 128], BF16) for i in range(2)]
        Y = [ps(f"Y{i}", [128, 512], F32) for i in range(4)]
        R0 = ps("R0", [128, 512], F32)
        F0 = ps("F0", [128, 512], F32)
        K_TR = ["ps_TR0", "ps_TR1"]
        K_Y = ["ps_Y0", "ps_Y1", "ps_Y2", "ps_Y3"]

        P = Prog(nc)
        out_ops = []
        _bc_cache = {}

        def _bc(e):
            if "r" not in _bc_cache:
                _bc_cache["r"] = e.to_reg(NSLOT - 1)
            return _bc_cache["r"]

        def g8k(i):
            return ("g8", i)

        wbig = BIG[:, 0:32768].rearrange("p (c n) -> p c n", n=2048)
        K_wbig = [("big", j) for j in range(4)]

        def ew(j):
            return BIG[:, j * 8192:(j + 1) * 8192]

        P.pool(lambda e: e.memset(identf[:], 1.0), writes=["identf"])
        P.pool(lambda e: e.affine_select(out=identf[:], in_=identf[:], pattern=[[-1, 128]],
                                         compare_op=ALU.is_equal, fill=0.0, base=0,
                                         channel_multiplier=1), reads=["identf"], writes=["identf"])
        P.dve(lambda e: e.tensor_copy(out=ident[:], in_=identf[:]), reads=["identf"], writes=["ident"])
        P.pool(lambda e: e.memset(Umat[:], 1.0), writes=["Umat"])
        P.pool(lambda e: e.affine_select(out=Umat[:], in_=Umat[:], pattern=[[1, 128]],
                                         compare_op=ALU.is_gt, fill=0.0, base=0,
                                         channel_multiplier=-1), reads=["Umat"], writes=["Umat"])
        P.pool(lambda e: e.memset(ONESf[:], 1.0), writes=["ONESf"])
        for i in range(6):
            P.pool(lambda e, i=i: e.memset(G8[i][:], 0.0), writes=[g8k(i)])
        P.pool(lambda e: e.memset(rs[:], 0.0), writes=["rs15"])
        P.pool(lambda e: e.memset(epsv[:], LN_EPS), writes=["epsv"])
        zsrc = G8[5][:].bitcast(BF16).rearrange("p (a d) -> p a d", d=2048)
        for r in range(NSLOT // 256):
            P.dma("sync", lambda e, r=r: e.dma_start(
                out=Xd[r * 256:(r + 1) * 256, :].rearrange("(a p) d -> p a d", p=128), in_=zsrc),
                reads=[g8k(5)], writes=[("Xdz", r)])
        for (dst, src, k) in [(br_sb, None, "br"), (ebase, ebase_d, "ebase"), (halo, halo_d, "halo"),
                              (bpw1, bpw1_d, "bpw1"), (wdw, wdw_d, "wdw"), (bdw, bdw_d, "bdw"),
                              (cg, cg_d, "cg"), (cb, cb_d, "cb")]:
            if src is None:
                continue
            P.dma("sync", lambda e, dst=dst, src=src: e.dma_start(out=dst[:], in_=src), writes=[k])

        def transpose_tile_bf16(src_ap_fn, src_keys, dst_fn, dst_keys, all_act=False):
            for half in range(2):
                for cc in range(8):
                    c = half * 8 + cc
                    P.pe(lambda e, c=c, cc=cc, half=half: e.transpose(out=TR[half][:, cc, :], in_=src_ap_fn(c),
                                                                      identity=ident[:]),
                         reads=list(src_keys) + ["ident"], writes=[K_TR[half]])
                if half == 0 or all_act:
                    P.act(lambda e, half=half: e.copy(out=dst_fn(half * 8), in_=TR[half][:]),
                          reads=[K_TR[half]], writes=list(dst_keys))
                else:
                    P.dve(lambda e, half=half: e.tensor_copy(out=dst_fn(half * 8), in_=TR[half][:]),
                          reads=[K_TR[half]], writes=list(dst_keys))

        def layer_norm(z, zk, gvec, bvec, gbk, out, outk):
            for j in range(4):
                P.dve(lambda e, j=j: e.bn_stats(out=stt6[:, j, :], in_=z[:, j * 512:(j + 1) * 512]),
                      reads=[zk], writes=[("stt6", j)])
            P.dve(lambda e: e.bn_aggr(out=mv[:], in_=stt6[:]), reads=[("stt6", j) for j in range(4)], writes=["mv"])
            P.act(lambda e: e.activation(out=lnsc[:, 0:1], in_=mv[:, 1:2], func=AF.Ln, bias=epsv[:, 0:1], scale=1.0),
                  reads=["mv", "epsv"], writes=["lnsc0"])
            P.act(lambda e: e.activation(out=lnsc[:, 0:1], in_=lnsc[:, 0:1], func=AF.Exp, scale=-0.5),
                  reads=["lnsc0"], writes=["lnsc0"])
            P.dve(lambda e: e.scalar_tensor_tensor(out=lnsc[:, 1:2], in0=mv[:, 0:1], scalar=-1.0, in1=lnsc[:, 0:1],
                                                   op0=ALU.mult, op1=ALU.mult), reads=["mv", "lnsc0"], writes=["lnsc1"])
            P.act(lambda e: e.activation(out=out[:], in_=z[:], func=AF.Identity, bias=lnsc[:, 1:2],
                                         scale=lnsc[:, 0:1]), reads=[zk, "lnsc0", "lnsc1"], writes=[outk])
            P.dve(lambda e: e.tensor_tensor(out=out[:], in0=out[:], in1=gvec[:], op=ALU.mult),
                  reads=[outk, gbk[0]], writes=[outk])
            P.dve(lambda e: e.tensor_tensor(out=out[:], in0=out[:], in1=bvec[:], op=ALU.add),
                  reads=[outk, gbk[1]], writes=[outk])

        RS = []
        for par in range(2):
            RS.append(dict(
                lg=sb(f"lg{par}", [128, 36], F32), rs=sb(f"rsx{par}", [128, 16], F32),
                oh_g=sb(f"oh_g{par}", [128, 4], F32), gexp=sb(f"gexp{par}", [128, 4], F32),
                esel=sb(f"esel{par}", [128, 8], F32), top8=sb(f"top8{par}", [128, 8], F32),
                m1=sb(f"m1{par}", [128, 8], F32), m12=sb(f"m12{par}", [128, 8], F32),
                ind32=sb(f"ind32{par}", [128, 32], F32), ind1=sb(f"ind1{par}", [128, 32], F32),
                ind2=sb(f"ind2{par}", [128, 32], F32)))

        def route_front(L, xm, xmk, ti, par, xmT, xmTk, xmb, xmbk, do_lg=True):
            S = RS[par]
            lg = S["lg"]
            for r4 in range(4):
                for k in range(4):
                    c = r4 * 4 + k
                    P.pe(lambda e, c=c, k=k: e.transpose(out=F0[:, k * 128:(k + 1) * 128],
                                                         in_=xm[:, c * 128:(c + 1) * 128], identity=identf[:]),
                         reads=[xmk, "identf"], writes=["ps_F0"])
                P.act(lambda e, r4=r4: e.copy(out=xmT[:, r4 * 4:(r4 + 1) * 4, :],
                                              in_=F0[:].rearrange("p (a b) -> p a b", b=128)),
                      reads=["ps_F0"], writes=[xmTk])
            for c in range(16):
                P.pe(lambda e, c=c: e.matmul(R0[:, 0:36], lhsT=xmT[:, c, :], rhs=wr_sb[:, c, :],
                                             start=(c == 0), stop=(c == 15)),
                     reads=[xmTk, "wr"], writes=["ps_R0"])
            if do_lg:
                route_lg(par)
            P.act(lambda e: e.copy(out=xmb[:], in_=xm[:]), reads=[xmk], writes=[xmbk])

        def route_lg(par):
            lg = RS[par]["lg"]
            P.dve(lambda e: e.tensor_tensor(out=lg[:], in0=R0[:, 0:36], in1=br_sb[:], op=ALU.add),
                  reads=["ps_R0", "br"], writes=[("lg", par)])

        def route_math(L, ti, par):
            S = RS[par]
            lg, rs_, oh_g, gexp, esel, top8, m1, m12 = (S["lg"], S["rs"], S["oh_g"], S["gexp"], S["esel"],
                                                        S["top8"], S["m1"], S["m12"])
            ind32, ind1, ind2 = S["ind32"], S["ind1"], S["ind2"]
            K = lambda n: (n, par)
            D = P.dve
            D(lambda e: e.tensor_reduce(out=rs_[:, 0:1], in_=lg[:, 0:4], axis=AX.X, op=ALU.max),
              reads=[K("lg")], writes=[K("rs0")])
            D(lambda e: e.tensor_scalar(out=oh_g[:], in0=lg[:, 0:4], scalar1=rs_[:, 0:1], scalar2=None, op0=ALU.is_ge),
              reads=[K("lg"), K("rs0")], writes=[K("oh_g")])
            D(lambda e: e.tensor_scalar(out=rs_[:, 1:2], in0=rs_[:, 0:1], scalar1=-1.0, scalar2=None, op0=ALU.mult),
              reads=[K("rs0")], writes=[K("rs1")])
            P.act(lambda e: e.activation(out=gexp[:], in_=lg[:, 0:4], func=AF.Exp, bias=rs_[:, 1:2], scale=1.0,
                                         accum_out=rs_[:, 2:3]), reads=[K("lg"), K("rs1")], writes=[K("gexp"), K("rs2")])
            D(lambda e: e.reciprocal(out=rs_[:, 3:4], in_=rs_[:, 2:3]), reads=[K("rs2")], writes=[K("rs3")])
            D(lambda e: e.tensor_scalar(out=esel[:], in0=lg[:, 4:12], scalar1=oh_g[:, 0:1], scalar2=None, op0=ALU.mult),
              reads=[K("lg"), K("oh_g")], writes=[K("esel")])
            for g in range(1, 4):
                D(lambda e, g=g: e.scalar_tensor_tensor(out=esel[:], in0=lg[:, 4 + 8 * g:12 + 8 * g],
                                                        scalar=oh_g[:, g:g + 1], in1=esel[:],
                                                        op0=ALU.mult, op1=ALU.add),
                  reads=[K("lg"), K("oh_g"), K("esel")], writes=[K("esel")])
            D(lambda e: e.max(out=top8[:], in_=esel[:]), reads=[K("esel")], writes=[K("top8")])
            D(lambda e: e.tensor_scalar(out=m1[:], in0=esel[:], scalar1=top8[:, 0:1], scalar2=None, op0=ALU.is_ge),
              reads=[K("esel"), K("top8")], writes=[K("m1")])
            D(lambda e: e.tensor_scalar(out=m12[:], in0=esel[:], scalar1=top8[:, 1:2], scalar2=None, op0=ALU.is_ge),
              reads=[K("esel"), K("top8")], writes=[K("m12")])
            D(lambda e: e.tensor_tensor(out=rs_[:, 4:5], in0=top8[:, 1:2], in1=top8[:, 0:1], op=ALU.subtract),
              reads=[K("top8")], writes=[K("rs4")])
            P.act(lambda e: e.activation(out=rs_[:, 5:6], in_=rs_[:, 4:5], func=AF.Exp), reads=[K("rs4")], writes=[K("rs5")])
            D(lambda e: e.tensor_scalar(out=rs_[:, 6:7], in0=rs_[:, 5:6], scalar1=1.0, scalar2=None, op0=ALU.add),
              reads=[K("rs5")], writes=[K("rs6")])
            D(lambda e: e.reciprocal(out=rs_[:, 7:8], in_=rs_[:, 6:7]), reads=[K("rs6")], writes=[K("rs7")])
            D(lambda e: e.tensor_tensor(out=rs_[:, 8:9], in0=rs_[:, 7:8], in1=rs_[:, 3:4], op=ALU.mult),
              reads=[K("rs7"), K("rs3")], writes=[K("rs8")])
            D(lambda e: e.tensor_tensor(out=rs_[:, 9:10], in0=rs_[:, 3:4], in1=rs_[:, 8:9], op=ALU.subtract),
              reads=[K("rs8"), K("rs3")], writes=[K("rs9")])
            for g in range(4):
                D(lambda e, g=g: e.tensor_scalar(out=ind32[:, 8 * g:8 * g + 8], in0=m12[:], scalar1=oh_g[:, g:g + 1],
                                                 scalar2=None, op0=ALU.mult),
                  reads=[K("m12"), K("oh_g")], writes=[("ind32", par, g)])
                D(lambda e, g=g: e.tensor_scalar(out=ind1[:, 8 * g:8 * g + 8], in0=m1[:], scalar1=oh_g[:, g:g + 1],
                                                 scalar2=None, op0=ALU.mult),
                  reads=[K("m1"), K("oh_g")], writes=[("ind1", par, g)])
            i32k = [("ind32", par, g) for g in range(4)]
            i1k = [("ind1", par, g) for g in range(4)]
            D(lambda e: e.tensor_tensor(out=ind2[:], in0=ind32[:], in1=ind1[:], op=ALU.subtract),
              reads=i32k + i1k, writes=[K("ind2")])

        def route_pos(L, ti, par, xmb, xmbk):
            route_pos_pe(L, ti, par)
            route_pos_rest(L, ti, par, xmb, xmbk)

        def route_pos_pe(L, ti, par):
            ind32 = RS[par]["ind32"]
            i32k = [("ind32", par, g) for g in range(4)]
            if ti == 0:
                P.pe(lambda e: e.matmul(R0[:, 64:96], lhsT=Umat[:], rhs=ind32[:], start=True, stop=True),
                     reads=["Umat"] + i32k, writes=["ps_R0"])
            else:
                P.pe(lambda e: e.matmul(R0[:, 64:96], lhsT=Umat[:], rhs=ind32[:], start=True, stop=False),
                     reads=["Umat"] + i32k, writes=["ps_R0"])
                P.pe(lambda e: e.matmul(R0[:, 64:96], lhsT=ONESf[:], rhs=indcum[:], start=False, stop=True),
                     reads=["ONESf", "indcum"], writes=["ps_R0"])

        def route_pos_rest(L, ti, par, xmb, xmbk):
            S = RS[par]
            rs_, ind32, ind1, ind2 = S["rs"], S["ind32"], S["ind1"], S["ind2"]
            idx1, idx2, wt1, wt2 = idx1a[:, L, :], idx2a[:, L, :], wt1a[:, L, :], wt2a[:, L, :]
            K = lambda n: (n, par)
            D = P.dve
            i32k = [("ind32", par, g) for g in range(4)]
            i1k = [("ind1", par, g) for g in range(4)]
            D(lambda e: e.tensor_tensor(out=slotv[:], in0=R0[:, 64:96], in1=ebase[:], op=ALU.add),
              reads=["ps_R0", "ebase"], writes=["slotv"])
            D(lambda e: e.tensor_scalar(out=valid[:], in0=R0[:, 64:96], scalar1=float(CAP), scalar2=None, op0=ALU.is_lt),
              reads=["ps_R0"], writes=["valid"])
            if ti == 0:
                D(lambda e: e.tensor_copy(out=indcum[:], in_=ind32[:]), reads=i32k, writes=["indcum"])
            else:
                D(lambda e: e.tensor_tensor(out=indcum[:], in0=indcum[:], in1=ind32[:], op=ALU.add),
                  reads=i32k + ["indcum"], writes=["indcum"])
            for (k, indk, indt, wcol, idxt, wtt) in [(0, i1k, ind1, 8, idx1, wt1), (1, [K("ind2")], ind2, 9, idx2, wt2)]:
                D(lambda e, indt=indt: e.tensor_tensor(out=tmp32[:], in0=indt[:], in1=slotv[:], op=ALU.mult),
                  reads=list(indk) + ["slotv"], writes=["tmp32"])
                D(lambda e, k=k: e.tensor_reduce(out=idxf[:, k:k + 1], in_=tmp32[:], axis=AX.X, op=ALU.add),
                  reads=["tmp32"], writes=[("idxf", k)])
                D(lambda e, indt=indt: e.tensor_tensor(out=tmp32[:], in0=indt[:], in1=valid[:], op=ALU.mult),
                  reads=list(indk) + ["valid", ("idxf", k)], writes=["tmp32"])
                D(lambda e, k=k: e.tensor_reduce(out=rs_[:, 10 + k:11 + k], in_=tmp32[:], axis=AX.X, op=ALU.add),
                  reads=["tmp32"], writes=[K(("ok", k))])
                D(lambda e, k=k, wcol=wcol, wtt=wtt: e.tensor_tensor(out=wtt[:, ti:ti + 1], in0=rs_[:, wcol:wcol + 1],
                                                                     in1=rs_[:, 10 + k:11 + k], op=ALU.mult),
                  reads=[K("rs8"), K("rs9"), K(("ok", k))], writes=[("wt", L, k, ti)])
                D(lambda e, k=k: e.tensor_scalar(out=rs_[:, 12 + k:13 + k], in0=rs_[:, 10 + k:11 + k], scalar1=-BIGIDX,
                                                 scalar2=BIGIDX, op0=ALU.mult, op1=ALU.add),
                  reads=[K(("ok", k))], writes=[K(("pen", k))])
                D(lambda e, k=k: e.tensor_tensor(out=idxf[:, k:k + 1], in0=idxf[:, k:k + 1], in1=rs_[:, 12 + k:13 + k],
                                                 op=ALU.add), reads=[("idxf", k), K(("pen", k))], writes=[("idxf", k)])
                D(lambda e, k=k, idxt=idxt: e.tensor_copy(out=idxt[:, ti:ti + 1], in_=idxf[:, k:k + 1]),
                  reads=[("idxf", k)], writes=[("idx", L, k, ti)])
            for (k, idxt) in [(0, idx1), (1, idx2)]:
                P.dma("gpsimd", lambda e, idxt=idxt: e.indirect_dma_start(
                    out=Xd, out_offset=bass.IndirectOffsetOnAxis(ap=idxt[:, ti:ti + 1], axis=0),
                    in_=xmb[:, :], in_offset=None, bounds_check=_bc(e), oob_is_err=False),
                    reads=[xmbk, ("idx", L, k, ti)] + ([("Xdz", r) for r in range(NSLOT // 256)] if (L == 0 and ti == 0) else []),
                    writes=[("Xd", L, ti, k)])

        def experts(L, ntiles):
            xd_keys = [("Xd", L, ti, k) for ti in range(ntiles) for k in range(2)]
            for E in range(NEXP):
                par = E % 2
                wg_v = ew(2 * par).rearrange("p (c f) -> p c f", f=512)
                wu_v = ew(2 * par + 1).rearrange("p (c f) -> p c f", f=512)
                wd_v = ew(4 + par).rearrange("p (c d) -> p c d", d=2048)
                kg, ku, kd = ("big", 2 * par), ("big", 2 * par + 1), ("big", 4 + par)
                P.dma("gpsimd", lambda e, E=E, wg_v=wg_v: e.dma_start(
                    out=wg_v, in_=wg_d[L, E].rearrange("(c p) f -> p c f", p=128)), writes=[kg])
                P.dma("gpsimd", lambda e, E=E, wu_v=wu_v: e.dma_start(
                    out=wu_v, in_=wu_d[L, E].rearrange("(c p) f -> p c f", p=128)), writes=[ku])
                P.dma("gpsimd", lambda e, E=E, wd_v=wd_v: e.dma_start(
                    out=wd_v, in_=wd_d[L, E].rearrange("(c p) d -> p c d", p=128)), writes=[kd])
                for sh in range(2):
                    P.dma("sync", lambda e, E=E, sh=sh: e.dma_start(
                        out=B4[sh][:], in_=Xd[E * CAP + sh * 128:E * CAP + (sh + 1) * 128, :]),
                        reads=xd_keys, writes=[("b4", sh)])
                for sh in range(2):
                    transpose_tile_bf16(lambda c, sh=sh: B4[sh][:, c * 128:(c + 1) * 128], [("b4", sh)],
                                        lambda c0, sh=sh: XET[:, c0:c0 + 8, sh * 128:(sh + 1) * 128], ["xet"])
                xetk = ["xet"]
                for (wv, wk, b0) in [(wg_v, kg, 0), (wu_v, ku, 2)]:
                    for fc in range(4):
                        bank = Y[b0 + fc // 2]
                        bk = K_Y[b0 + fc // 2]
                        for c in range(16):
                            P.pe(lambda e, wv=wv, fc=fc, c=c, bank=bank: e.matmul(
                                bank[:, (fc % 2) * 256:(fc % 2) * 256 + 256], lhsT=wv[:, c, fc * 128:(fc + 1) * 128],
                                rhs=XET[:, c, :], start=(c == 0), stop=(c == 15)),
                                reads=[wk] + xetk, writes=[bk])
                SG = G8[5]
                for b in range(2):
                    P.act(lambda e, b=b: e.activation(out=SG[:, b * 512:(b + 1) * 512], in_=Y[b][:], func=AF.Silu),
                          reads=[K_Y[b]], writes=[g8k(5)])
                    P.dve(lambda e, b=b: e.tensor_tensor(
                        out=HT[:, 2 * b:2 * b + 2, :], in0=SG[:, b * 512:(b + 1) * 512].rearrange("p (a b) -> p a b", b=256),
                        in1=Y[2 + b][:].rearrange("p (a b) -> p a b", b=256), op=ALU.mult),
                        reads=[g8k(5), K_Y[2 + b]], writes=[("ht", b)])
                htk = [("ht", 0), ("ht", 1)]
                for sh in range(2):
                    ye = G8[3 + sh]
                    for dc in range(4):
                        db = (sh * 4 + dc) % 2
                        bank, bk = (R0, "ps_R0") if db == 0 else (F0, "ps_F0")
                        for fc in range(4):
                            P.pe(lambda e, fc=fc, sh=sh, dc=dc, bank=bank, wd_v=wd_v: e.matmul(
                                bank[:], lhsT=HT[:, fc, sh * 128:(sh + 1) * 128], rhs=wd_v[:, fc, dc * 512:(dc + 1) * 512],
                                start=(fc == 0), stop=(fc == 3)), reads=htk + [kd], writes=[bk])
                        if dc % 2 == 0:
                            P.act(lambda e, ye=ye, dc=dc, bank=bank: e.copy(out=ye[:, dc * 512:(dc + 1) * 512], in_=bank[:]),
                                  reads=[bk], writes=[g8k(3 + sh)])
                        else:
                            P.dve(lambda e, ye=ye, dc=dc, bank=bank: e.tensor_copy(out=ye[:, dc * 512:(dc + 1) * 512], in_=bank[:]),
                                  reads=[bk], writes=[g8k(3 + sh)])
                    P.dma("sync", lambda e, E=E, sh=sh, ye=ye: e.dma_start(
                        out=Yd[E * CAP + sh * 128:E * CAP + (sh + 1) * 128, :], in_=ye[:]),
                        reads=[g8k(3 + sh)], writes=[("Yd", L, E)])

        def combine_tile(L, ti, ntot_exp, xm_src, gi, dest_fn):
            ydk = [("Yd", L, E) for E in range(ntot_exp)]
            idx1, idx2, wt1, wt2 = idx1a[:, L, :], idx2a[:, L, :], wt1a[:, L, :], wt2a[:, L, :]
            ya, yb = (0, 1) if ti % 2 == 0 else (4, 5)
            y1, y2, xm, z = G8[ya], G8[yb], G8[2], G8[3]
            for (k, idxt, yt, yk) in [(0, idx1, y1, g8k(ya)), (1, idx2, y2, g8k(yb))]:
                P.dma("gpsimd", lambda e, idxt=idxt, yt=yt: e.indirect_dma_start(
                    out=yt[:, :], out_offset=None, in_=Yd,
                    in_offset=bass.IndirectOffsetOnAxis(ap=idxt[:, ti:ti + 1], axis=0),
                    bounds_check=_bc(e), oob_is_err=False),
                    reads=ydk + [("idx", L, k, ti)], writes=[yk])
            P.dma("sync", lambda e: e.dma_start(out=xm[:], in_=xm_src), reads=[("XMs", L, ti)], writes=[g8k(2)])
            P.dve(lambda e: e.tensor_scalar(out=y1[:], in0=y1[:], scalar1=wt1[:, ti:ti + 1], scalar2=None, op0=ALU.mult),
                  reads=[g8k(ya), ("wt", L, 0, ti)], writes=[g8k(ya)])
            P.dve(lambda e: e.scalar_tensor_tensor(out=y1[:], in0=y2[:], scalar=wt2[:, ti:ti + 1], in1=y1[:],
                                                   op0=ALU.mult, op1=ALU.add),
                  reads=[g8k(ya), g8k(yb), ("wt", L, 1, ti)], writes=[g8k(ya)])
            P.dve(lambda e: e.scalar_tensor_tensor(out=z[:], in0=xm[:], scalar=ALPHA, in1=y1[:],
                                                   op0=ALU.mult, op1=ALU.add),
                  reads=[g8k(ya), g8k(2)], writes=[g8k(3)])
            layer_norm(z, g8k(3), gvecs[gi][0], gvecs[gi][1], gvecs[gi][2], xm, g8k(2))
            return dest_fn(xm, g8k(2))

        GV = [sb("GVg", [128, 2048], F32), sb("GVb", [128, 2048], F32)]
        gvecs = {}

        def load_gb(gi):
            P.dma("sync", lambda e: e.dma_start(out=GV[0][:], in_=lng_d[gi]), writes=["GVg"])
            P.dma("sync", lambda e: e.dma_start(out=GV[1][:], in_=lnb_d[gi]), writes=["GVb"])
            gvecs[gi] = (GV[0], GV[1], ("GVg", "GVb"))

        def load_router(L):
            P.dma("sync", lambda e: e.dma_start(out=wr_sb[:], in_=wr_d[L].rearrange("(c p) n -> p c n", p=128)),
                  writes=["wr"])
            P.dma("sync", lambda e: e.dma_start(out=br_sb[:], in_=br_d[L]), writes=["br"])

        P.dma("gpsimd", lambda e: e.dma_start(out=wbig, in_=wo_d.rearrange("(c p) n -> p c n", p=128)), writes=K_wbig)
        load_gb(0)
        load_router(0)
        xmT_v = G8[5][:].rearrange("p (c t) -> p c t", t=128)
        def b1_load(ti):
            ab = B4[ti % 2]
            xt = G8[ti % 2]
            P.dma("gpsimd", lambda e, ti=ti, ab=ab: e.dma_start(out=ab[:], in_=ain[ti * 128:(ti + 1) * 128, :]),
                  writes=[("b4", ti % 2)])
            P.dma("sync", lambda e, ti=ti, xt=xt: e.dma_start(out=xt[:], in_=xin[ti * 128:(ti + 1) * 128, :]),
                  writes=[g8k(ti % 2)])

        def b1_a1(ti):
            ab = B4[ti % 2]
            transpose_tile_bf16(lambda c, ab=ab: ab[:, c * 128:(c + 1) * 128], [("b4", ti % 2)],
                                lambda c0: T16[:, c0:c0 + 8, :], ["xet"], all_act=True)
            for dc in range(4):
                for c in range(16):
                    P.pe(lambda e, dc=dc, c=c: e.matmul(Y[dc][:], lhsT=T16[:, c, :], rhs=wbig[:, c, dc * 512:(dc + 1) * 512],
                                                        start=(c == 0), stop=(c == 15)),
                         reads=["xet"] + K_wbig, writes=[K_Y[dc]])

        def b1_z(ti):
            xt = G8[ti % 2]
            z = G8[2]
            for dc in range(4):
                P.dve(lambda e, dc=dc, xt=xt: e.scalar_tensor_tensor(
                    out=z[:, dc * 512:(dc + 1) * 512], in0=xt[:, dc * 512:(dc + 1) * 512], scalar=ALPHA, in1=Y[dc][:],
                    op0=ALU.mult, op1=ALU.add), reads=[g8k(ti % 2), K_Y[dc]], writes=[g8k(2)])

        def b1_ln(ti):
            xm = G8[3 + ti % 2]
            xmk = g8k(3 + ti % 2)
            layer_norm(G8[2], g8k(2), gvecs[0][0], gvecs[0][1], gvecs[0][2], xm, xmk)
            P.dma("sync", lambda e, ti=ti, xm=xm: e.dma_start(out=XM[0, ti * 128:(ti + 1) * 128, :], in_=xm[:]),
                  reads=[xmk], writes=[("XMs", 0, ti)])

        b1_load(0)
        b1_load(1)
        for j in range(NTL + 3):
            if 0 <= j - 3 < NTL:
                route_pos_pe(0, j - 3, (j - 3) % 2)
            if 0 <= j - 1 < NTL:
                b1_z(j - 1)
            if j < NTL:
                b1_a1(j)
            if 0 <= j - 1 < NTL:
                if j + 1 < NTL:
                    b1_load(j + 1)
                b1_ln(j - 1)
            if 0 <= j - 3 < NTL:
                t_ = j - 3
                route_pos_rest(0, t_, t_ % 2, B4[2 + t_ % 2], ("b4", 2 + t_ % 2))
            if 0 <= j - 2 < NTL:
                route_lg((j - 2) % 2)
            if 0 <= j - 1 < NTL:
                t_ = j - 1
                route_front(0, G8[3 + t_ % 2], g8k(3 + t_ % 2), t_, t_ % 2, xmT_v, g8k(5),
                            B4[2 + t_ % 2], ("b4", 2 + t_ % 2), do_lg=False)
            if 0 <= j - 2 < NTL:
                t_ = j - 2
                route_math(0, t_, t_ % 2)
        experts(0, NTL)
        load_gb(1)

        def dest_l0(ti):
            def f(xt, xk):
                return P.dma("sync", lambda e: e.dma_start(out=X1[ti * 128:(ti + 1) * 128, :], in_=xt[:]),
                             reads=[xk], writes=[("X1s", ti)])
            return f
        for ti in range(NTL):
            o = combine_tile(0, ti, NEXP, XM[0, ti * 128:(ti + 1) * 128, :], 1, dest_l0(ti))
            if not do_layer1 and ti >= 1:
                out_ops.append(P.dma("sync", lambda e, ti=ti: e.dma_start(out=out_d[(ti - 1) * 128:ti * 128, :], in_=G8[2][:]),
                                     reads=[g8k(2)]))
        if do_layer1:
            NCH = NMAIN // 2
            CW = 384
            X1T = BIG[:, 32768:32768 + 16 * CW].rearrange("p (c t) -> p c t", t=CW)
            XMT1 = BIG[:, 40960:45056].bitcast(F32).rearrange("p (c t) -> p c t", t=128)
            ALLK = [("big", 4), ("big", 5)]
            P.dve(lambda e: e.tensor_copy(out=rs[:, 15:16], in_=rs[:, 15:16]), reads=["rs15"] + ALLK,
                  writes=["rs15", "xmt1"] + [("dg", k) for k in range(31)] + ALLK + [("x1t", j) for j in range(3)] + [("st", cc) for cc in range(16)])
            P.dma("gpsimd", lambda e: e.dma_start(out=wbig, in_=wpw2_d.rearrange("(c p) n -> p c n", p=128)),
                  writes=K_wbig)
            load_gb(2)
            load_router(1)
            UT = [G8[4], G8[5]]

            def ut(cc):
                return UT[cc // 8][:, (cc % 8) * 256:(cc % 8 + 1) * 256]

            def utk(cc):
                return g8k(4 + cc // 8)
            ACC = sb("ACC", [128, 2, 256], F32)
            HTc = [sb(f"HTc{i}", [128, CW], BF16) for i in range(2)]
            SGc = sb("SGc", [128, CW], F32)
            DG = BIG[:, 45056:45056 + 31 * 128].rearrange("p (k c) -> p k c", c=128)
            USQ = [sb(f"USQ{i}", [128, 512], BF16) for i in range(2)]
            ONESb = sb("ONESb", [128, 128], BF16)
            P.dve(lambda e: e.tensor_copy(out=ONESb[:], in_=ONESf[:]), reads=["ONESf"], writes=["ONESb"])
            MR = sb("MR", [128, 2, 256], F32)
            wbufs = [
                (B4[2][:].rearrange("p (c n) -> p c n", n=128), ("b4", 2),
                 B4[3][:].rearrange("p (c n) -> p c n", n=128), ("b4", 3)),
                (XETf[:, 0:2048].rearrange("p (c n) -> p c n", n=128), "xet",
                 XETf[:, 2048:4096].rearrange("p (c n) -> p c n", n=128), "xet"),
            ]
            for q in range(NCH):
                x1tk = [("x1t", j) for j in range(3)]
                stk = [("st", cc) for cc in range(16)]
                P.dve(lambda e: e.tensor_copy(out=rs[:, 15:16], in_=rs[:, 15:16]), reads=["rs15"],
                      writes=["rs15"] + x1tk + stk)
                for j in range(3):
                    gt = 2 * q + j
                    ab = B4[j % 2]
                    P.dma("gpsimd", lambda e, gt=gt, ab=ab: e.dma_start(out=ab[:], in_=X1[gt * 128:(gt + 1) * 128, :]),
                          reads=[("X1s", gt)], writes=[("b4", j % 2)])
                    transpose_tile_bf16(lambda c, ab=ab: ab[:, c * 128:(c + 1) * 128], [("b4", j % 2)],
                                        lambda c0, j=j: X1T[:, c0:c0 + 8, j * 128:(j + 1) * 128],
                                        [("x1t", j)])
                def stage_ag(cc):
                    wch, wk, gch, gk = wbufs[cc % 2]
                    ba = (cc % 2) * 2
                    P.dma("gpsimd", lambda e, cc=cc, wch=wch: e.dma_start(
                        out=wch, in_=wpw1_d[:, cc * 128:(cc + 1) * 128].rearrange("(c p) n -> p c n", p=128)),
                        writes=[wk])
                    P.dma("gpsimd", lambda e, cc=cc, gch=gch: e.dma_start(
                        out=gch, in_=wpw1_d[:, 2048 + cc * 128:2048 + (cc + 1) * 128].rearrange("(c p) n -> p c n", p=128)),
                        writes=[gk])
                    for (wv, wkk, bi) in [(wch, wk, ba), (gch, gk, ba + 1)]:
                        for c in range(16):
                            P.pe(lambda e, wv=wv, c=c, bi=bi: e.matmul(
                                Y[bi][:, 0:CW], lhsT=wv[:, c, :], rhs=X1T[:, c, :],
                                start=(c == 0), stop=(c == 15)), reads=[wkk] + x1tk, writes=[K_Y[bi]])
                    hT = HTc[cc % 2]
                    hk = ("htc", cc % 2)
                    P.act(lambda e, cc=cc, ba=ba: e.activation(out=SGc[:], in_=Y[ba + 1][:, 0:CW], func=AF.Sigmoid,
                                                               bias=bpw1[:, 16 + cc:17 + cc], scale=1.0),
                          reads=[K_Y[ba + 1], "bpw1"], writes=["sgc"])
                    P.dve(lambda e, cc=cc, hT=hT, ba=ba: e.scalar_tensor_tensor(
                        out=hT[:], in0=Y[ba][:, 0:CW], scalar=bpw1[:, cc:cc + 1], in1=SGc[:],
                        op0=ALU.add, op1=ALU.mult), reads=[K_Y[ba], "sgc", "bpw1"], writes=[hk])
                    if q == 0:
                        P.dve(lambda e, hT=hT: e.tensor_scalar(out=hT[:, 0:128], in0=hT[:, 0:128], scalar1=halo[:, 0:1],
                                                               scalar2=None, op0=ALU.mult),
                              reads=[hk, "halo"], writes=[hk])

                def stage_dg(cc):
                    for k in range(31):
                        if k % 2 == 0:
                            P.act(lambda e, cc=cc, k=k: e.activation(out=DG[:, k, :], in_=ident[:], func=AF.Identity,
                                                                     scale=wdw[:, cc, k:k + 1]),
                                  reads=["ident", "wdw"], writes=[("dg", k)])
                        else:
                            P.dve(lambda e, cc=cc, k=k: e.tensor_scalar(out=DG[:, k, :], in0=ident[:],
                                                                        scalar1=wdw[:, cc, k:k + 1], scalar2=None,
                                                                        op0=ALU.mult),
                                  reads=["ident", "wdw"], writes=[("dg", k)])

                def stage_conv(cc):
                    hT = HTc[cc % 2]
                    hk = ("htc", cc % 2)
                    u = ut(cc)
                    for k in range(31):
                        P.pe(lambda e, k=k, hT=hT: e.matmul(
                            R0[:, 0:256], lhsT=DG[:, k, :], rhs=hT[:, 98 + k:98 + k + 256],
                            start=(k == 0), stop=(k == 30)), reads=[("dg", k), hk], writes=["ps_R0"])
                    P.act(lambda e, cc=cc, u=u: e.activation(out=u, in_=R0[:, 0:256], func=AF.Identity,
                                                             bias=bdw[:, cc:cc + 1], scale=1.0),
                          reads=["ps_R0", "bdw"], writes=[utk(cc)])
                    P.act(lambda e, u=u, cc=cc: e.activation(out=USQ[cc % 2][:, 256:512], in_=u, func=AF.Square),
                          reads=[utk(cc)], writes=[("usq", cc % 2)])
                    P.dve(lambda e, u=u, cc=cc: e.tensor_copy(out=USQ[cc % 2][:, 0:256], in_=u),
                          reads=[utk(cc)], writes=[("ub", cc % 2)])

                def stage_stats(cc):
                    u = ut(cc)
                    P.pe(lambda e, cc=cc: e.matmul(F0[:], lhsT=ONESb[:], rhs=USQ[cc % 2][:], start=True, stop=True),
                         reads=["ONESb", ("usq", cc % 2), ("ub", cc % 2)], writes=["ps_F0"])
                    if cc == 0:
                        P.dve(lambda e: e.tensor_copy(out=ACC[:].rearrange("p a b -> p (a b)"), in_=F0[:]),
                              reads=["ps_F0"], writes=["acc0", "acc1"])
                    else:
                        P.dve(lambda e: e.tensor_tensor(out=ACC[:].rearrange("p a b -> p (a b)"),
                                                        in0=ACC[:].rearrange("p a b -> p (a b)"), in1=F0[:], op=ALU.add),
                              reads=["ps_F0", "acc0", "acc1"], writes=["acc0", "acc1"])

                for i in range(18):
                    if 0 <= i - 1 < 16:
                        stage_dg(i - 1)
                    if i < 16:
                        stage_ag(i)
                    if 0 <= i - 1 < 16:
                        stage_conv(i - 1)
                    if 0 <= i - 2 < 16:
                        stage_stats(i - 2)
                P.dve(lambda e: e.tensor_scalar(out=MR[:, 0, :], in0=ACC[:, 0, :], scalar1=1.0 / 2048, scalar2=None,
                                                op0=ALU.mult), reads=["acc0"], writes=["mr0"])
                P.dve(lambda e: e.tensor_tensor(out=MR[:, 1, :], in0=MR[:, 0, :], in1=MR[:, 0, :], op=ALU.mult),
                      reads=["mr0"], writes=["mr1"])
                P.dve(lambda e: e.scalar_tensor_tensor(out=MR[:, 1, :], in0=ACC[:, 1, :], scalar=1.0 / 2048, in1=MR[:, 1, :],
                                                       op0=ALU.mult, op1=ALU.subtract),
                      reads=["acc1", "mr1"], writes=["mr1"])
                P.act(lambda e: e.activation(out=MR[:, 1, :], in_=MR[:, 1, :], func=AF.Ln, bias=epsv[:, 0:1], scale=1.0),
                      reads=["mr1", "epsv"], writes=["mr1"])
                P.act(lambda e: e.activation(out=MR[:, 1, :], in_=MR[:, 1, :], func=AF.Exp, scale=-0.5),
                      reads=["mr1"], writes=["mr1"])
                for cc in range(16):
                    u = ut(cc)
                    eng = P.dve
                    eng(lambda e, u=u: e.tensor_tensor(out=u, in0=u, in1=MR[:, 0, :], op=ALU.subtract),
                        reads=[utk(cc), "mr0"], writes=[utk(cc)])
                    eng(lambda e, u=u: e.tensor_tensor(out=u, in0=u, in1=MR[:, 1, :], op=ALU.mult),
                        reads=[utk(cc), "mr1"], writes=[utk(cc)])
                    P.act(lambda e, u=u, cc=cc: e.activation(out=X1T[:, cc, 0:256], in_=u, func=AF.Silu,
                                                            bias=cb[:, cc:cc + 1], scale=cg[:, cc:cc + 1]),
                          reads=[utk(cc), "cg", "cb"], writes=[("st", cc)] + x1tk)
                xm_bufs = [(G8[3], g8k(3)), (G8[4], g8k(4))]
                for t in range(2):
                    gt = 2 * q + 1 + t
                    l1t = gt - 1
                    xt = G8[t % 2]
                    P.dma("sync", lambda e, gt=gt, xt=xt: e.dma_start(out=xt[:], in_=X1[gt * 128:(gt + 1) * 128, :]),
                          reads=[("X1s", gt)], writes=[g8k(t % 2)])
                for t in range(2):
                    gt = 2 * q + 1 + t
                    l1t = gt - 1
                    xt = G8[t % 2]
                    z = G8[2]
                    for dc in range(4):
                        for cc in range(16):
                            P.pe(lambda e, dc=dc, cc=cc, t=t: e.matmul(
                                Y[dc][:], lhsT=X1T[:, cc, t * 128:(t + 1) * 128], rhs=wbig[:, cc, dc * 512:(dc + 1) * 512],
                                start=(cc == 0), stop=(cc == 15)), reads=stk + K_wbig, writes=[K_Y[dc]])
                        P.dve(lambda e, dc=dc, xt=xt: e.scalar_tensor_tensor(
                            out=z[:, dc * 512:(dc + 1) * 512], in0=xt[:, dc * 512:(dc + 1) * 512], scalar=ALPHA,
                            in1=Y[dc][:], op0=ALU.mult, op1=ALU.add), reads=[g8k(t % 2), K_Y[dc]], writes=[g8k(2)])
                    xm, xmk = xm_bufs[t]
                    layer_norm(z, g8k(2), gvecs[2][0], gvecs[2][1], gvecs[2][2], xm, xmk)
                    P.dma("sync", lambda e, l1t=l1t, xm=xm: e.dma_start(out=XM[1, l1t * 128:(l1t + 1) * 128, :], in_=xm[:]),
                          reads=[xmk], writes=[("XMs", 1, l1t)])
                for t in range(2):
                    l1t = 2 * q + t
                    xm, xmk = xm_bufs[t]
                    route_front(1, xm, xmk, l1t, t, XMT1, "xmt1", B4[t % 2], ("b4", t % 2))
                    route_math(1, l1t, t)
                for t in range(2):
                    l1t = 2 * q + t
                    route_pos(1, l1t, t, B4[t % 2], ("b4", t % 2))
            P.dve(lambda e: e.tensor_copy(out=rs[:, 15:16], in_=rs[:, 15:16]), reads=["rs15"],
                  writes=["rs15", "xmt1"] + [("dg", k) for k in range(31)] + ALLK + [("x1t", j) for j in range(3)] + [("st", cc) for cc in range(16)])
            experts(1, NMAIN)
            load_gb(3)

            def dest_l1(ti):
                def f(xt, xk):
                    return P.dma("sync", lambda e: e.dma_start(out=out_d[ti * 128:(ti + 1) * 128, :], in_=xt[:]),
                                 reads=[xk])
                return f
            for ti in range(NMAIN):
                o = combine_tile(1, ti, NEXP, XM[1, ti * 128:(ti + 1) * 128, :], 3, dest_l1(ti))
                out_ops.append(o)
        P.finish(out_ops)
        stats = P.build(st)
        print("rest stats", stats)
    return nc


N_CORES = 8
SEQ = 16384


def _attn_core_inputs(core, x2d, wqkv):
    w = np.empty((2, 2048, 384), np.float32)
    for j in range(2):
        h = 2 * core + j
        w[j, :, 0:128] = wqkv[:, h * 128:(h + 1) * 128]
        w[j, :, 128:256] = wqkv[:, 2048 + h * 128:2048 + (h + 1) * 128]
        w[j, :, 256:384] = wqkv[:, 4096 + h * 128:4096 + (h + 1) * 128]
    bt, fc = attn_consts(core)
    return {"x": x2d, "wqkv": w, "btab": bt, "fcorr": fc}


def _rest_core_inputs(d, core, x_full, attn_o, NTL):
    T = NTL - 1
    t0 = core * T * 128

    def halo_slice(a):
        out = np.zeros((NTL * 128, a.shape[1]), np.float32)
        if t0 >= 128:
            out[:] = a[t0 - 128:t0 + T * 128]
        else:
            out[128:] = a[t0:t0 + T * 128]
        return out

    def rep(v):
        return np.ascontiguousarray(np.broadcast_to(v[None, :], (128, v.shape[0]))).astype(np.float32)

    def fm(v, n):
        return np.ascontiguousarray(v.reshape(n, 128).T)

    inp = {}
    inp["xin"] = halo_slice(x_full)
    inp["ain"] = halo_slice(attn_o)
    inp["wo"] = d["attn_w_o"][0]
    inp["lng"] = np.stack([rep(d["mix_ln_g"][0]), rep(d["ffn_ln_g"][0]), rep(d["mix_ln_g"][1]), rep(d["ffn_ln_g"][1])])
    inp["lnb"] = np.stack([rep(d["mix_ln_b"][0]), rep(d["ffn_ln_b"][0]), rep(d["mix_ln_b"][1]), rep(d["ffn_ln_b"][1])])
    inp["wr"] = np.ascontiguousarray(np.concatenate([d["moe_w_grp"], d["moe_w_rt"].reshape(2, 2048, 32)], axis=2))
    br = np.concatenate([d["moe_b_grp"], d["moe_b_rt"].reshape(2, 32)], axis=1)
    inp["br"] = np.stack([rep(br[0]), rep(br[1])])
    inp["wgate"] = d["moe_w_gate"].reshape(2, 32, 2048, 512)
    inp["wup"] = d["moe_w_up"].reshape(2, 32, 2048, 512)
    inp["wdown"] = d["moe_w_down"].reshape(2, 32, 512, 2048)
    inp["wpw1"] = d["conv_w_pw1"][0]
    inp["bpw1"] = fm(d["conv_b_pw1"][0], 32)
    inp["wdw"] = np.ascontiguousarray(d["conv_w_dw"][0].reshape(31, 16, 128).transpose(2, 1, 0))
    inp["bdw"] = fm(d["conv_b_dw"][0], 16)
    inp["cg"] = fm(d["conv_ln_g"][0], 16)
    inp["cb"] = fm(d["conv_ln_b"][0], 16)
    inp["wpw2"] = d["conv_w_pw2"][0]
    inp["halo"] = np.full((128, 1), 0.0 if core == 0 else 1.0, np.float32)
    inp["ebase"] = rep(np.arange(32, dtype=np.float32) * CAP)
    return inp


def kernel(**inputs):
    d = {k: np.asarray(v, dtype=np.float32) for k, v in inputs.items()}
    x2d = np.ascontiguousarray(d["x"][0])
    wqkv = d["attn_w_qkv"][0]
    nc_a = build_attn(SEQ // 512)
    res_a = run_bass_kernel_spmd(nc_a, [_attn_core_inputs(c, x2d, wqkv) for c in range(N_CORES)],
                                 core_ids=list(range(N_CORES)))
    attn_o = np.concatenate([r["o"] for r in res_a.results], axis=1)
    NTL = SEQ // N_CORES // 128 + 1
    nc_b = build_rest(NTL, 32, True)
    res_b = run_bass_kernel_spmd(nc_b, [_rest_core_inputs(d, c, x2d, attn_o, NTL) for c in range(N_CORES)],
                                 core_ids=list(range(N_CORES)))
    out = np.concatenate([r["out"] for r in res_b.results], axis=0)
    return out.reshape(1, SEQ, 2048).astype(np.float32)
```

```python
from contextlib import ExitStack
import numpy as np
import concourse.bass as bass
import concourse.mybir as mybir
from concourse.bass_utils import run_bass_kernel_spmd


F32 = mybir.dt.float32
BF16 = mybir.dt.bfloat16
I32 = mybir.dt.int32
AF = mybir.ActivationFunctionType
ALU = mybir.AluOpType
AX = mybir.AxisListType

ENGS = ["tensor", "vector", "scalar", "gpsimd", "sync"]
EPOCH = 3000
N_DMA_SEMS = 36
DMA_POOLS = {"sync": (0, 12), "gpsimd": (12, 24), "scalar": (24, 36), "vector": (24, 36), "tensor": (24, 36)}


class Op:
    __slots__ = ("eng", "fn", "reads", "writes", "dma", "deps", "needs_inc",
                 "inc_sem", "inc_val", "idx", "dma_slot")

    def __init__(self, eng, fn, reads, writes, dma):
        self.eng = eng
        self.fn = fn
        self.reads = tuple(reads)
        self.writes = tuple(writes)
        self.dma = dma
        self.deps = []
        self.needs_inc = False
        self.inc_sem = None
        self.inc_val = None
        self.dma_slot = None


class Prog:
    def __init__(self, nc, same_engine_sync=True):
        self.nc = nc
        self.ops = []
        self.same_engine_sync = same_engine_sync
        self.final_waits = []

    @staticmethod
    def is_psum(k):
        if isinstance(k, tuple):
            k = k[0]
        return isinstance(k, str) and k.startswith("ps_")

    def op(self, eng, fn, reads=(), writes=(), dma=False):
        o = Op(eng, fn, reads, writes, dma)
        o.idx = len(self.ops)
        self.ops.append(o)
        return o

    def pe(self, fn, reads=(), writes=()):
        return self.op("tensor", fn, reads, writes)

    def dve(self, fn, reads=(), writes=()):
        return self.op("vector", fn, reads, writes)

    def act(self, fn, reads=(), writes=()):
        return self.op("scalar", fn, reads, writes)

    def pool(self, fn, reads=(), writes=()):
        return self.op("gpsimd", fn, reads, writes)

    def dma(self, eng, fn, reads=(), writes=()):
        return self.op(eng, fn, reads, writes, dma=True)

    def finish(self, out_ops):
        self.final_waits = list(out_ops)

    def build(self, stack, semstack=None, prefix=""):
        if semstack is None:
            semstack = stack
        nc = self.nc
        last_writer = {}
        readers = {}
        dma_rr = {e: DMA_POOLS[e][0] for e in ENGS}
        dma_last = [None] * N_DMA_SEMS
        for o in self.ops:
            deps = set()
            for r in o.reads:
                w = last_writer.get(r)
                if w is not None:
                    deps.add(w)
                if self.is_psum(r):
                    for rd in readers.get(r, ()):
                        if rd.eng != o.eng:
                            deps.add(rd)
            for wk in o.writes:
                w = last_writer.get(wk)
                if w is not None:
                    deps.add(w)
                for rd in readers.get(wk, ()):
                    deps.add(rd)
            deps.discard(o)
            if o.dma:
                slot = dma_rr[o.eng]
                o.dma_slot = slot
                prev = dma_last[slot]
                if prev is not None:
                    deps.add(prev)
                dma_last[slot] = o
                lo, hi = DMA_POOLS[o.eng]
                dma_rr[o.eng] = lo + (slot + 1 - lo) % (hi - lo)
            final = []
            for d in deps:
                if (not d.dma) and d.eng == o.eng and not o.dma:
                    if o.eng == "tensor" or not self.same_engine_sync:
                        continue
                if (not d.dma) and d.eng == o.eng and o.dma and o.eng == "tensor":
                    continue
                final.append(d)
                d.needs_inc = True
            o.deps = final
            for r in o.reads:
                readers.setdefault(r, []).append(o)
            for wk in o.writes:
                last_writer[wk] = o
                readers[wk] = []
        for d in self.final_waits:
            d.needs_inc = True
        cnt = {e: 0 for e in ENGS}
        dma_cnt = [0] * N_DMA_SEMS
        n_epochs = {e: 1 for e in ENGS}
        for o in self.ops:
            if o.dma:
                dma_cnt[o.dma_slot] += 1
                o.inc_sem = ("dma", o.dma_slot)
                o.inc_val = 16 * dma_cnt[o.dma_slot]
            elif o.needs_inc:
                cnt[o.eng] += 1
                ep = (cnt[o.eng] - 1) // EPOCH
                o.inc_sem = (o.eng, ep)
                o.inc_val = cnt[o.eng] - ep * EPOCH
                n_epochs[o.eng] = max(n_epochs[o.eng], ep + 1)
        sems = {}
        for e in ENGS:
            for ep in range(n_epochs[e]):
                sems[(e, ep)] = semstack.enter_context(nc.semaphore(f"{prefix}c_{e}_{ep}"))
        for i in range(N_DMA_SEMS):
            sems[("dma", i)] = semstack.enter_context(nc.semaphore(f"{prefix}d_{i}"))
        block = stack.enter_context(nc.Block())
        by_eng = {e: [o for o in self.ops if o.eng == e] for e in ENGS}
        final_waits = self.final_waits
        stats = {e: [0, 0] for e in ENGS}

        def emit_engine(e, eng):
            waited = {}
            for o in by_eng[e]:
                for d in o.deps:
                    k = d.inc_sem
                    if waited.get(k, 0) >= d.inc_val:
                        continue
                    eng.wait_ge(sems[k], d.inc_val)
                    waited[k] = d.inc_val
                    stats[e][1] += 1
                ins = o.fn(eng)
                stats[e][0] += 1
                if o.dma:
                    ins.then_inc(sems[o.inc_sem], 16)
                elif o.needs_inc:
                    ins.then_inc(sems[o.inc_sem], 1)
            if e == "sync":
                for i in range(N_DMA_SEMS):
                    if dma_cnt[i] > 0 and waited.get(("dma", i), 0) < 16 * dma_cnt[i]:
                        eng.wait_ge(sems[("dma", i)], 16 * dma_cnt[i])
                        waited[("dma", i)] = 16 * dma_cnt[i]
                for d in final_waits:
                    k = d.inc_sem
                    if waited.get(k, 0) >= d.inc_val:
                        continue
                    eng.wait_ge(sems[k], d.inc_val)
                    waited[k] = d.inc_val

        @block.tensor
        def _(eng):
            emit_engine("tensor", eng)

        @block.vector
        def _(eng):
            emit_engine("vector", eng)

        @block.scalar
        def _(eng):
            emit_engine("scalar", eng)

        @block.gpsimd
        def _(eng):
            emit_engine("gpsimd", eng)

        @block.sync
        def _(eng):
            emit_engine("sync", eng)

        self.stats = stats
        return stats


NBT = 272
ATT_SCALE = 128.0 ** -0.5


def attn_consts(core):
    bt = np.zeros((2, 128, NBT), np.float32)
    fc = np.zeros((2, 128, 4), np.float32)
    p = np.arange(128, dtype=np.float64)[:, None]
    m = (np.arange(NBT, dtype=np.float64) - 8.0)[None, :]
    for j in range(2):
        h = 2 * core + j
        slope = 2.0 ** (-8.0 * (h + 1) / 16.0)
        bt[j] = (slope * (p - 64.0 * m)).astype(np.float32)
        for t in range(4):
            fc[j, :, t] = np.float32(np.exp(-slope * (320.0 + 128.0 * t)))
    return bt, fc


def build_attn(NG):
    S = NG * 512
    nc = bass.Bass("TRN2", target_bir_lowering=False)
    x_d = nc.dram_tensor("x", [S, 2048], F32, kind="ExternalInput").ap()
    w_d = nc.dram_tensor("wqkv", [2, 2048, 384], F32, kind="ExternalInput").ap()
    bt_d = nc.dram_tensor("btab", [2, 128, NBT], F32, kind="ExternalInput").ap()
    fc_d = nc.dram_tensor("fcorr", [2, 128, 4], F32, kind="ExternalInput").ap()
    o_d = nc.dram_tensor("o", [S, 256], F32, kind="ExternalOutput").ap()
    NT = S // 128
    with ExitStack() as st:
        def sb(name, shape, dt):
            return st.enter_context(nc.sbuf_tensor(name, shape, dt))

        def ps(name, shape, dt):
            return st.enter_context(nc.psum_tensor(name, shape, dt))

        w_sb = sb("w_sb", [128, 16, 384], BF16)
        xb = [sb(f"xb{i}", [128, 2048], BF16) for i in range(8)]
        xT = sb("xT", [128, 16, 512], BF16)
        qT = [sb(f"qT{i}", [128, 512], BF16) for i in range(2)]
        qf = sb("qf", [128, 512], F32)
        kT = sb("kT", [128, S], BF16)
        Vp = sb("Vp", [128, NT, 129], BF16)
        ksum = sb("ksum", [128, 64], F32)
        gate = sb("gate", [128, 4, 64], F32)
        mx = sb("mx", [128, 4, 8], F32)
        W = [sb(f"W{i}", [128, 4, 64], F32) for i in range(2)]
        NPT = 4
        pT = [sb(f"pT{i}", [128, 512], BF16) for i in range(NPT)]
        Oacc = [sb(f"Oacc{i}", [128, 4, 129], F32) for i in range(2)]
        rden = sb("rden", [128, 4], F32)
        ost = [sb(f"ost{i}", [128, 4, 128], F32) for i in range(2)]
        identf = sb("identf", [128, 128], F32)
        ident = sb("ident", [128, 128], BF16)
        trif = sb("trif", [128, 128], F32)
        tri = sb("tri", [128, 128], BF16)
        btab = sb("btab_sb", [128, NBT], F32)
        fcorr = sb("fcorr_sb", [128, 4], F32)

        NS = 4
        s_ps = [ps(f"s{i}", [128, 512], F32) for i in range(NS)]
        po = [[ps(f"po{i}{j}", [128, 512], F32) for j in range(2)] for i in range(2)]
        tr = [s_ps[i][:].bitcast(BF16).rearrange("p (a b) -> p a b", b=128) for i in range(2)]
        pq, pk, pv, pg = po[0][0], po[0][1], po[1][0], po[1][1]
        K_pq, K_pk, K_pv, K_pg = "ps_po00", "ps_po01", "ps_po10", "ps_po11"

        P = Prog(nc)
        P.pool(lambda e: e.memset(identf[:], 1.0), writes=["identf"])
        P.pool(lambda e: e.affine_select(out=identf[:], in_=identf[:], pattern=[[-1, 128]],
                                         compare_op=ALU.is_equal, fill=0.0, base=0,
                                         channel_multiplier=1), reads=["identf"], writes=["identf"])
        P.dve(lambda e: e.tensor_copy(out=ident[:], in_=identf[:]), reads=["identf"], writes=["ident"])
        P.pool(lambda e: e.memset(trif[:], 1.0), writes=["trif"])
        P.pool(lambda e: e.affine_select(out=trif[:], in_=trif[:], pattern=[[1, 128]],
                                         compare_op=ALU.is_ge, fill=0.0, base=0,
                                         channel_multiplier=-1), reads=["trif"], writes=["trif"])
        P.dve(lambda e: e.tensor_copy(out=tri[:], in_=trif[:]), reads=["trif"], writes=["tri"])
        P.dve(lambda e: e.memset(ksum[:], 0.0), writes=[("ksum", gg) for gg in range(NG)])
        P.dve(lambda e: e.memset(Vp[:, :, 128:129], 1.0), writes=["Vones"])

        out_ops = []
        pt_rr = [0]
        sq_rr = [0]
        bank_cnt = {}
        bank_tot = {}
        blk_par = [0]

        for hh in range(2):
            P.dve(lambda e: e.memset(gate[:], -1e30), writes=[("gate", t) for t in range(4)])
            P.dma("gpsimd", lambda e, hh=hh: e.dma_start(
                out=w_sb[:], in_=w_d[hh].rearrange("(c p) n -> p c n", p=128)), writes=["w"])
            P.dma("sync", lambda e, hh=hh: e.dma_start(out=btab[:], in_=bt_d[hh]), writes=["btab"])
            P.dma("sync", lambda e, hh=hh: e.dma_start(out=fcorr[:], in_=fc_d[hh]), writes=["fcorr"])
            for g in range(NG):
                gp = g % 2
                for i in range(4):
                    bi = gp * 4 + i
                    tok0 = g * 512 + i * 128
                    P.dma("gpsimd", lambda e, bi=bi, tok0=tok0: e.dma_start(
                        out=xb[bi][:], in_=x_d[tok0:tok0 + 128, :]), writes=[("xb", bi)])
                for i in range(4):
                    bi = gp * 4 + i
                    for half in range(2):
                        tb = half
                        for cc in range(8):
                            c = half * 8 + cc
                            P.pe(lambda e, bi=bi, c=c, tb=tb, cc=cc: e.transpose(
                                out=tr[tb][:, cc, :], in_=xb[bi][:, c * 128:(c + 1) * 128], identity=ident[:]),
                                reads=[("xb", bi), "ident"], writes=[("ps_s", tb)])
                        if half == 0:
                            P.act(lambda e, tb=tb, half=half, i=i: e.copy(
                                out=xT[:, half * 8:(half + 1) * 8, i * 128:(i + 1) * 128], in_=tr[tb]),
                                reads=[("ps_s", tb)], writes=[("xT", i, half)])
                        else:
                            P.dve(lambda e, tb=tb, half=half, i=i: e.tensor_copy(
                                out=xT[:, half * 8:(half + 1) * 8, i * 128:(i + 1) * 128], in_=tr[tb]),
                                reads=[("ps_s", tb)], writes=[("xT", i, half)])
                xT_keys = [("xT", i, half) for i in range(4) for half in range(2)]
                for c in range(16):
                    P.pe(lambda e, c=c: e.matmul(pq[:], lhsT=w_sb[:, c, 0:128], rhs=xT[:, c, :],
                                                 start=(c == 0), stop=(c == 15)),
                         reads=["w"] + xT_keys, writes=[K_pq])
                for c in range(16):
                    P.pe(lambda e, c=c: e.matmul(pk[:], lhsT=w_sb[:, c, 128:256], rhs=xT[:, c, :],
                                                 start=(c == 0), stop=(c == 15)),
                         reads=["w"] + xT_keys, writes=[K_pk])
                for i in range(4):
                    for c in range(16):
                        P.pe(lambda e, c=c, i=i: e.matmul(pv[:, i * 128:(i + 1) * 128],
                                                          lhsT=xT[:, c, i * 128:(i + 1) * 128],
                                                          rhs=w_sb[:, c, 256:384],
                                                          start=(c == 0), stop=(c == 15)),
                             reads=["w"] + xT_keys, writes=[K_pv])
                P.act(lambda e, gp=gp: e.copy(out=qT[gp][:], in_=pq[:]), reads=[K_pq], writes=[("qT", gp)])
                P.dve(lambda e: e.tensor_copy(out=qf[:], in_=pq[:]), reads=[K_pq], writes=["qf"])
                P.act(lambda e, g=g: e.copy(out=kT[:, g * 512:(g + 1) * 512], in_=pk[:]),
                      reads=[K_pk], writes=[("kT", g)])
                P.dve(lambda e, g=g: e.tensor_reduce(
                    out=ksum[:, 2 * g:2 * g + 2], in_=pk[:].rearrange("p (a b) -> p a b", b=256),
                    axis=AX.X, op=ALU.add), reads=[K_pk], writes=[("ksum", g)])
                P.dve(lambda e, g=g: e.tensor_copy(
                    out=Vp[:, 4 * g:4 * g + 4, 0:128], in_=pv[:].rearrange("p (a b) -> p a b", b=128)),
                    reads=[K_pv], writes=[("V", g)])
                ksum_keys = [("ksum", gg) for gg in range(g + 1)]
                for t in range(4):
                    own = 2 * g + t // 2
                    if own == 0:
                        continue
                    P.pe(lambda e, t=t: e.matmul(pg[:, t * 64:(t + 1) * 64], lhsT=qf[:, t * 128:(t + 1) * 128],
                                                 rhs=ksum[:, 0:64], start=True, stop=True),
                         reads=["qf"] + ksum_keys, writes=[K_pg])
                for t in range(4):
                    own = 2 * g + t // 2
                    if own == 0:
                        continue
                    P.dve(lambda e, t=t, own=own: e.tensor_copy(out=gate[:, t, 0:own], in_=pg[:, t * 64:t * 64 + own]),
                          reads=[K_pg], writes=[("gate", t)])
                    P.dve(lambda e, t=t: e.max(out=mx[:, t, :], in_=gate[:, t, :]),
                          reads=[("gate", t)], writes=[("mx", t)])
                    P.dve(lambda e, t=t, gp=gp: e.tensor_scalar(
                        out=W[gp][:, t, :], in0=gate[:, t, :], scalar1=mx[:, t, 2:3], scalar2=None, op0=ALU.is_ge),
                        reads=[("gate", t), ("mx", t)], writes=[("W", gp, t)])
                    nfar = 2 * g - 1
                    if nfar > 0:
                        P.dve(lambda e, t=t, gp=gp, nfar=nfar: e.tensor_scalar(
                            out=W[gp][:, t, 0:nfar], in0=W[gp][:, t, 0:nfar], scalar1=fcorr[:, t:t + 1],
                            scalar2=None, op0=ALU.mult),
                            reads=[("W", gp, t), "fcorr"], writes=[("W", gp, t)])

                jobs = []

                def do_tile(kt, acts, pv_list, masked_sub=None):
                    rec = {"si": None}
                    cmin = min(a[0] for a in acts)

                    def emit_qk(kt=kt, cmin=cmin, rec=rec, gp=gp):
                        si = sq_rr[0] % NS
                        sq_rr[0] += 1
                        rec["si"] = si
                        P.pe(lambda e, kt=kt, si=si, cmin=cmin, gp=gp: e.matmul(
                            s_ps[si][:, cmin:512], lhsT=kT[:, kt * 128:(kt + 1) * 128], rhs=qT[gp][:, cmin:512],
                            start=True, stop=True),
                            reads=[("kT", kt // 4), ("qT", gp)], writes=[("ps_s", si)])

                    def emit_rest(kt=kt, acts=acts, pv_list=pv_list, masked_sub=masked_sub, rec=rec):
                        si = rec["si"]
                        pi = pt_rr[0] % NPT
                        pt_rr[0] += 1
                        for (c0, c1, m) in acts:
                            P.act(lambda e, si=si, pi=pi, c0=c0, c1=c1, m=m: e.activation(
                                out=pT[pi][:, c0:c1], in_=s_ps[si][:, c0:c1], func=AF.Exp,
                                bias=btab[:, m + 8:m + 9], scale=ATT_SCALE),
                                reads=[("ps_s", si), "btab"],
                                writes=[("pT", pi, ss) for ss in range(c0 // 128, c1 // 128)])
                        if masked_sub is not None:
                            c0 = masked_sub * 128
                            P.pool(lambda e, pi=pi, c0=c0: e.tensor_tensor(
                                out=pT[pi][:, c0:c0 + 128], in0=pT[pi][:, c0:c0 + 128], in1=tri[:], op=ALU.mult),
                                reads=[("pT", pi, masked_sub), "tri"], writes=[("pT", pi, masked_sub)])
                        for (t, po_t, pkey, stt, stp) in pv_list:
                            cnt_ = bank_cnt[pkey]
                            bank_cnt[pkey] += 1
                            stt = (cnt_ == 0)
                            stp = (cnt_ == bank_tot[pkey] - 1)
                            P.pe(lambda e, t=t, po_t=po_t, pi=pi, kt=kt, stt=stt, stp=stp: e.matmul(
                                po_t[:, (t % 2) * 129:(t % 2) * 129 + 129], lhsT=pT[pi][:, t * 128:(t + 1) * 128],
                                rhs=Vp[:, kt, :], start=stt, stop=stp),
                                reads=[("pT", pi, t), ("V", kt // 4), "Vones"], writes=[pkey])
                    jobs.append(("tile", emit_qk, emit_rest))

                def po_for(t, bp):
                    return po[bp][t // 2], f"ps_po{bp}{t // 2}"

                def new_round(bp, n0=4, n1=4):
                    def f(bp=bp, n0=n0, n1=n1):
                        bank_cnt[f"ps_po{bp}0"] = 0
                        bank_cnt[f"ps_po{bp}1"] = 0
                        bank_tot[f"ps_po{bp}0"] = n0
                        bank_tot[f"ps_po{bp}1"] = n1
                    jobs.append(("call", f))

                def accum(t, bp, mode, wcol=None):
                    jobs.append(("call", lambda t=t, bp=bp, mode=mode, wcol=wcol: accum_now(t, bp, mode, wcol)))

                def accum_now(t, bp, mode, wcol=None):
                    po_t, pkey = po_for(t, bp)
                    src = po_t[:, (t % 2) * 129:(t % 2) * 129 + 129]
                    if mode == "init":
                        P.dve(lambda e, t=t, src=src, gp=gp: e.tensor_copy(out=Oacc[gp][:, t, :], in_=src),
                              reads=[pkey], writes=[("Oacc", gp, t)])
                    else:
                        P.dve(lambda e, t=t, src=src, wcol=wcol, gp=gp: e.scalar_tensor_tensor(
                            out=Oacc[gp][:, t, :], in0=src, scalar=W[gp][:, t, wcol:wcol + 1],
                            in1=Oacc[gp][:, t, :], op0=ALU.mult, op1=ALU.add),
                            reads=[pkey, ("W", gp, t), ("Oacc", gp, t)], writes=[("Oacc", gp, t)])

                k0 = 4 * g
                bp = blk_par[0] % 2
                blk_par[0] += 1
                new_round(bp, 0, 3)
                p2, k2 = po_for(2, bp)
                do_tile(k0 + 2, [(256, 384, 1), (384, 512, 3)],
                        [(2, p2, k2, True, True), (3, p2, k2, True, False)], masked_sub=2)
                do_tile(k0 + 3, [(384, 512, 1)], [(3, p2, k2, False, True)], masked_sub=3)
                accum(2, bp, "init")
                accum(3, bp, "init")
                bp = blk_par[0] % 2
                blk_par[0] += 1
                new_round(bp, 3, 4)
                pa, ka = po_for(0, bp)
                pb_, kb_ = po_for(2, bp)
                do_tile(k0 + 0, [(0, 128, 1), (128, 256, 3), (256, 384, 5), (384, 512, 7)],
                        [(0, pa, ka, True, True), (1, pa, ka, True, False),
                         (2, pb_, kb_, True, False), (3, pb_, kb_, True, False)], masked_sub=0)
                do_tile(k0 + 1, [(128, 256, 1), (256, 384, 3), (384, 512, 5)],
                        [(1, pa, ka, False, True), (2, pb_, kb_, False, True), (3, pb_, kb_, False, True)],
                        masked_sub=1)
                accum(0, bp, "init")
                accum(1, bp, "init")
                accum(2, bp, "acc", wcol=2 * g)
                accum(3, bp, "acc", wcol=2 * g)
                if g >= 1:
                    n = 2 * g - 1
                    bp = blk_par[0] % 2
                    blk_par[0] += 1
                    new_round(bp)
                    for jj in range(2):
                        kt = 2 * n + jj
                        rel = kt - k0
                        acts = [(s * 128, (s + 1) * 128, 2 * (s - rel) + 1) for s in range(4)]
                        pvl = []
                        for t in range(4):
                            p_t, k_t = po_for(t, bp)
                            pvl.append((t, p_t, k_t, jj == 0, jj == 1))
                        do_tile(kt, acts, pvl)
                    for t in range(4):
                        accum(t, bp, "acc", wcol=n)
                for n in range(0, 2 * g - 1):
                    bp = blk_par[0] % 2
                    blk_par[0] += 1
                    new_round(bp)
                    for jj in range(2):
                        kt = 2 * n + jj
                        m = 2 * (k0 - 2 - kt)
                        pvl = []
                        for t in range(4):
                            p_t, k_t = po_for(t, bp)
                            pvl.append((t, p_t, k_t, jj == 0, jj == 1))
                        do_tile(kt, [(0, 512, m)], pvl)
                    for t in range(4):
                        accum(t, bp, "acc", wcol=n)
                LOOK = NS - 1
                tile_idx = [i for i, j in enumerate(jobs) if j[0] == "tile"]
                qk_done = 0
                for pos, job in enumerate(jobs):
                    if job[0] == "call":
                        job[1]()
                        continue
                    my = tile_idx.index(pos)
                    while qk_done < len(tile_idx) and qk_done <= my + LOOK:
                        jobs[tile_idx[qk_done]][1]()
                        qk_done += 1
                    job[2]()
                oacc_keys = [("Oacc", gp, t) for t in range(4)]
                P.dve(lambda e, gp=gp: e.reciprocal(out=rden[:], in_=Oacc[gp][:, :, 128]),
                      reads=oacc_keys, writes=["rden"])
                for t in range(4):
                    P.dve(lambda e, gp=gp, t=t: e.tensor_scalar(
                        out=ost[gp][:, t, :], in0=Oacc[gp][:, t, 0:128], scalar1=rden[:, t:t + 1],
                        scalar2=None, op0=ALU.mult),
                        reads=[("Oacc", gp, t), "rden"], writes=[("ost", gp)])
                oo = P.dma("sync", lambda e, gp=gp, g=g, hh=hh: e.dma_start(
                    out=o_d[g * 512:(g + 1) * 512, hh * 128:(hh + 1) * 128].rearrange("(t p) d -> p t d", p=128),
                    in_=ost[gp][:]), reads=[("ost", gp)])
                out_ops.append(oo)
        P.finish(out_ops)
        stats = P.build(st)
        print("attn stats", stats)
    return nc


ALPHA = float((2.0 * 2) ** 0.25)
LN_EPS = 1e-5
CAP = 256
NSLOT = 32 * CAP
BIGIDX = 1.0e6


def build_rest(NTL=17, NEXP=32, do_layer1=True):
    NMAIN = NTL - 1
    NQ = NMAIN // 4
    nc = bass.Bass("TRN2", target_bir_lowering=False)

    def din(name, shape, dt=F32):
        return nc.dram_tensor(name, shape, dt, kind="ExternalInput").ap()

    xin = din("xin", [NTL * 128, 2048])
    ain = din("ain", [NTL * 128, 2048])
    wo_d = din("wo", [2048, 2048])
    lng_d = din("lng", [4, 128, 2048])
    lnb_d = din("lnb", [4, 128, 2048])
    wr_d = din("wr", [2, 2048, 36])
    br_d = din("br", [2, 128, 36])
    wg_d = din("wgate", [2, 32, 2048, 512])
    wu_d = din("wup", [2, 32, 2048, 512])
    wd_d = din("wdown", [2, 32, 512, 2048])
    wpw1_d = din("wpw1", [2048, 4096])
    bpw1_d = din("bpw1", [128, 32])
    wdw_d = din("wdw", [128, 16, 31])
    bdw_d = din("bdw", [128, 16])
    cg_d = din("cg", [128, 16])
    cb_d = din("cb", [128, 16])
    wpw2_d = din("wpw2", [2048, 2048])
    halo_d = din("halo", [128, 1])
    ebase_d = din("ebase", [128, 32])
    out_d = nc.dram_tensor("out", [NMAIN * 128, 2048], F32, kind="ExternalOutput").ap()
    XM = nc.dram_tensor("XM", [2, NTL * 128, 2048], F32, kind="Internal").ap()
    X1 = nc.dram_tensor("X1", [NTL * 128, 2048], F32, kind="Internal").ap()
    Xd = nc.dram_tensor("Xd", [NSLOT, 2048], BF16, kind="Internal").ap()
    Yd = nc.dram_tensor("Yd", [NSLOT, 2048], F32, kind="Internal").ap()

    with ExitStack() as st:
        def sb(name, shape, dt):
            return st.enter_context(nc.sbuf_tensor(name, shape, dt))

        def ps(name, shape, dt):
            return st.enter_context(nc.psum_tensor(name, shape, dt))

        BIG = sb("BIG", [128, 49152], BF16)
        G8 = [sb(f"G8_{i}", [128, 2048], F32) for i in range(6)]
        B4 = [sb(f"B4_{i}", [128, 2048], BF16) for i in range(4)]
        XET = sb("XET", [128, 16, 256], BF16)
        XETf = XET[:].rearrange("p c t -> p (c t)")
        T16 = XETf[:, 0:2048].rearrange("p (c t) -> p c t", t=128)
        HT = sb("HT", [128, 4, 256], BF16)
        identf = sb("identf", [128, 128], F32)
        ident = sb("ident", [128, 128], BF16)
        Umat = sb("Umat", [128, 128], F32)
        ONESf = sb("ONESf", [128, 128], F32)
        wr_sb = sb("wr_sb", [128, 16, 36], F32)
        br_sb = sb("br_sb", [128, 36], F32)
        ebase = sb("ebase_sb", [128, 32], F32)
        halo = sb("halo_sb", [128, 1], F32)
        bpw1 = sb("bpw1_sb", [128, 32], F32)
        wdw = sb("wdw_sb", [128, 16, 31], F32)
        bdw = sb("bdw_sb", [128, 16], F32)
        cg = sb("cg_sb", [128, 16], F32)
        cb = sb("cb_sb", [128, 16], F32)
        idx1a = sb("idx1", [128, 2, NTL], I32)
        idx2a = sb("idx2", [128, 2, NTL], I32)
        wt1a = sb("wt1", [128, 2, NTL], F32)
        wt2a = sb("wt2", [128, 2, NTL], F32)
        indcum = sb("indcum", [128, 32], F32)
        rs = sb("rs", [128, 16], F32)
        slotv = sb("slotv", [128, 32], F32)
        valid = sb("valid", [128, 32], F32)
        tmp32 = sb("tmp32", [128, 32], F32)
        idxf = sb("idxf", [128, 2], F32)
        stt6 = sb("stt6", [128, 4, 6], F32)
        mv = sb("mv", [128, 2], F32)
        lnsc = sb("lnsc", [128, 2], F32)
        epsv = sb("epsv", [128, 1], F32)

        TR = [ps(f"TR{i}", [128, 8, 128], BF16) for i in range(2)]
        Y = [ps(f"Y{i}", [128, 512], F32) for i in range(4)]
        R0 = ps("R0", [128, 512], F32)
        F0 = ps("F0", [128, 512], F32)
        K_TR = ["ps_TR0", "ps_TR1"]
        K_Y = ["ps_Y0", "ps_Y1", "ps_Y2", "ps_Y3"]

        P = Prog(nc)
        out_ops = []
        _bc_cache = {}

        def _bc(e):
            if "r" not in _bc_cache:
                _bc_cache["r"] = e.to_reg(NSLOT - 1)
            return _bc_cache["r"]

        def g8k(i):
            return ("g8", i)

        wbig = BIG[:, 0:32768].rearrange("p (c n) -> p c n", n=2048)
        K_wbig = [("big", j) for j in range(4)]

        def ew(j):
            return BIG[:, j * 8192:(j + 1) * 8192]

        P.pool(lambda e: e.memset(identf[:], 1.0), writes=["identf"])
        P.pool(lambda e: e.affine_select(out=identf[:], in_=identf[:], pattern=[[-1, 128]],
                                         compare_op=ALU.is_equal, fill=0.0, base=0,
                                         channel_multiplier=1), reads=["identf"], writes=["identf"])
        P.dve(lambda e: e.tensor_copy(out=ident[:], in_=identf[:]), reads=["identf"], writes=["ident"])
        P.pool(lambda e: e.memset(Umat[:], 1.0), writes=["Umat"])
        P.pool(lambda e: e.affine_select(out=Umat[:], in_=Umat[:], pattern=[[1, 128]],
                                         compare_op=ALU.is_gt, fill=0.0, base=0,
                                         channel_multiplier=-1), reads=["Umat"], writes=["Umat"])
        P.pool(lambda e: e.memset(ONESf[:], 1.0), writes=["ONESf"])
        for i in range(6):
            P.pool(lambda e, i=i: e.memset(G8[i][:], 0.0), writes=[g8k(i)])
        P.pool(lambda e: e.memset(rs[:], 0.0), writes=["rs15"])
        P.pool(lambda e: e.memset(epsv[:], LN_EPS), writes=["epsv"])
        zsrc = G8[5][:].bitcast(BF16).rearrange("p (a d) -> p a d", d=2048)
        for r in range(NSLOT // 256):
            P.dma("sync", lambda e, r=r: e.dma_start(
                out=Xd[r * 256:(r + 1) * 256, :].rearrange("(a p) d -> p a d", p=128), in_=zsrc),
                reads=[g8k(5)], writes=[("Xdz", r)])
        for (dst, src, k) in [(br_sb, None, "br"), (ebase, ebase_d, "ebase"), (halo, halo_d, "halo"),
                              (bpw1, bpw1_d, "bpw1"), (wdw, wdw_d, "wdw"), (bdw, bdw_d, "bdw"),
                              (cg, cg_d, "cg"), (cb, cb_d, "cb")]:
            if src is None:
                continue
            P.dma("sync", lambda e, dst=dst, src=src: e.dma_start(out=dst[:], in_=src), writes=[k])

        def transpose_tile_bf16(src_ap_fn, src_keys, dst_fn, dst_keys, all_act=False):
            for half in range(2):
                for cc in range(8):
                    c = half * 8 + cc
                    P.pe(lambda e, c=c, cc=cc, half=half: e.transpose(out=TR[half][:, cc, :], in_=src_ap_fn(c),
                                                                      identity=ident[:]),
                         reads=list(src_keys) + ["ident"], writes=[K_TR[half]])
                if half == 0 or all_act:
                    P.act(lambda e, half=half: e.copy(out=dst_fn(half * 8), in_=TR[half][:]),
                          reads=[K_TR[half]], writes=list(dst_keys))
                else:
                    P.dve(lambda e, half=half: e.tensor_copy(out=dst_fn(half * 8), in_=TR[half][:]),
                          reads=[K_TR[half]], writes=list(dst_keys))

        def layer_norm(z, zk, gvec, bvec, gbk, out, outk):
            for j in range(4):
                P.dve(lambda e, j=j: e.bn_stats(out=stt6[:, j, :], in_=z[:, j * 512:(j + 1) * 512]),
                      reads=[zk], writes=[("stt6", j)])
            P.dve(lambda e: e.bn_aggr(out=mv[:], in_=stt6[:]), reads=[("stt6", j) for j in range(4)], writes=["mv"])
            P.act(lambda e: e.activation(out=lnsc[:, 0:1], in_=mv[:, 1:2], func=AF.Ln, bias=epsv[:, 0:1], scale=1.0),
                  reads=["mv", "epsv"], writes=["lnsc0"])
            P.act(lambda e: e.activation(out=lnsc[:, 0:1], in_=lnsc[:, 0:1], func=AF.Exp, scale=-0.5),
                  reads=["lnsc0"], writes=["lnsc0"])
            P.dve(lambda e: e.scalar_tensor_tensor(out=lnsc[:, 1:2], in0=mv[:, 0:1], scalar=-1.0, in1=lnsc[:, 0:1],
                                                   op0=ALU.mult, op1=ALU.mult), reads=["mv", "lnsc0"], writes=["lnsc1"])
            P.act(lambda e: e.activation(out=out[:], in_=z[:], func=AF.Identity, bias=lnsc[:, 1:2],
                                         scale=lnsc[:, 0:1]), reads=[zk, "lnsc0", "lnsc1"], writes=[outk])
            P.dve(lambda e: e.tensor_tensor(out=out[:], in0=out[:], in1=gvec[:], op=ALU.mult),
                  reads=[outk, gbk[0]], writes=[outk])
            P.dve(lambda e: e.tensor_tensor(out=out[:], in0=out[:], in1=bvec[:], op=ALU.add),
                  reads=[outk, gbk[1]], writes=[outk])

        RS = []
        for par in range(2):
            RS.append(dict(
                lg=sb(f"lg{par}", [128, 36], F32), rs=sb(f"rsx{par}", [128, 16], F32),
                oh_g=sb(f"oh_g{par}", [128, 4], F32), gexp=sb(f"gexp{par}", [128, 4], F32),
                esel=sb(f"esel{par}", [128, 8], F32), top8=sb(f"top8{par}", [128, 8], F32),
                m1=sb(f"m1{par}", [128, 8], F32), m12=sb(f"m12{par}", [128, 8], F32),
                ind32=sb(f"ind32{par}", [128, 32], F32), ind1=sb(f"ind1{par}", [128, 32], F32),
                ind2=sb(f"ind2{par}", [128, 32], F32)))

        def route_front(L, xm, xmk, ti, par, xmT, xmTk, xmb, xmbk, do_lg=True):
            S = RS[par]
            lg = S["lg"]
            for r4 in range(4):
                for k in range(4):
                    c = r4 * 4 + k
                    P.pe(lambda e, c=c, k=k: e.transpose(out=F0[:, k * 128:(k + 1) * 128],
                                                         in_=xm[:, c * 128:(c + 1) * 128], identity=identf[:]),
                         reads=[xmk, "identf"], writes=["ps_F0"])
                P.act(lambda e, r4=r4: e.copy(out=xmT[:, r4 * 4:(r4 + 1) * 4, :],
                                              in_=F0[:].rearrange("p (a b) -> p a b", b=128)),
                      reads=["ps_F0"], writes=[xmTk])
            for c in range(16):
                P.pe(lambda e, c=c: e.matmul(R0[:, 0:36], lhsT=xmT[:, c, :], rhs=wr_sb[:, c, :],
                                             start=(c == 0), stop=(c == 15)),
                     reads=[xmTk, "wr"], writes=["ps_R0"])
            if do_lg:
                route_lg(par)
            P.act(lambda e: e.copy(out=xmb[:], in_=xm[:]), reads=[xmk], writes=[xmbk])

        def route_lg(par):
            lg = RS[par]["lg"]
            P.dve(lambda e: e.tensor_tensor(out=lg[:], in0=R0[:, 0:36], in1=br_sb[:], op=ALU.add),
                  reads=["ps_R0", "br"], writes=[("lg", par)])

        def route_math(L, ti, par):
            S = RS[par]
            lg, rs_, oh_g, gexp, esel, top8, m1, m12 = (S["lg"], S["rs"], S["oh_g"], S["gexp"], S["esel"],
                                                        S["top8"], S["m1"], S["m12"])
            ind32, ind1, ind2 = S["ind32"], S["ind1"], S["ind2"]
            K = lambda n: (n, par)
            D = P.dve
            D(lambda e: e.tensor_reduce(out=rs_[:, 0:1], in_=lg[:, 0:4], axis=AX.X, op=ALU.max),
              reads=[K("lg")], writes=[K("rs0")])
            D(lambda e: e.tensor_scalar(out=oh_g[:], in0=lg[:, 0:4], scalar1=rs_[:, 0:1], scalar2=None, op0=ALU.is_ge),
              reads=[K("lg"), K("rs0")], writes=[K("oh_g")])
            D(lambda e: e.tensor_scalar(out=rs_[:, 1:2], in0=rs_[:, 0:1], scalar1=-1.0, scalar2=None, op0=ALU.mult),
              reads=[K("rs0")], writes=[K("rs1")])
            P.act(lambda e: e.activation(out=gexp[:], in_=lg[:, 0:4], func=AF.Exp, bias=rs_[:, 1:2], scale=1.0,
                                         accum_out=rs_[:, 2:3]), reads=[K("lg"), K("rs1")], writes=[K("gexp"), K("rs2")])
            D(lambda e: e.reciprocal(out=rs_[:, 3:4], in_=rs_[:, 2:3]), reads=[K("rs2")], writes=[K("rs3")])
            D(lambda e: e.tensor_scalar(out=esel[:], in0=lg[:, 4:12], scalar1=oh_g[:, 0:1], scalar2=None, op0=ALU.mult),
              reads=[K("lg"), K("oh_g")], writes=[K("esel")])
            for g in range(1, 4):
                D(lambda e, g=g: e.scalar_tensor_tensor(out=esel[:], in0=lg[:, 4 + 8 * g:12 + 8 * g],
                                                        scalar=oh_g[:, g:g + 1], in1=esel[:],
                                                        op0=ALU.mult, op1=ALU.add),
                  reads=[K("lg"), K("oh_g"), K("esel")], writes=[K("esel")])
            D(lambda e: e.max(out=top8[:], in_=esel[:]), reads=[K("esel")], writes=[K("top8")])
            D(lambda e: e.tensor_scalar(out=m1[:], in0=esel[:], scalar1=top8[:, 0:1], scalar2=None, op0=ALU.is_ge),
              reads=[K("esel"), K("top8")], writes=[K("m1")])
            D(lambda e: e.tensor_scalar(out=m12[:], in0=esel[:], scalar1=top8[:, 1:2], scalar2=None, op0=ALU.is_ge),
              reads=[K("esel"), K("top8")], writes=[K("m12")])
            D(lambda e: e.tensor_tensor(out=rs_[:, 4:5], in0=top8[:, 1:2], in1=top8[:, 0:1], op=ALU.subtract),
              reads=[K("top8")], writes=[K("rs4")])
            P.act(lambda e: e.activation(out=rs_[:, 5:6], in_=rs_[:, 4:5], func=AF.Exp), reads=[K("rs4")], writes=[K("rs5")])
            D(lambda e: e.tensor_scalar(out=rs_[:, 6:7], in0=rs_[:, 5:6], scalar1=1.0, scalar2=None, op0=ALU.add),
              reads=[K("rs5")], writes=[K("rs6")])
            D(lambda e: e.reciprocal(out=rs_[:, 7:8], in_=rs_[:, 6:7]), reads=[K("rs6")], writes=[K("rs7")])
            D(lambda e: e.tensor_tensor(out=rs_[:, 8:9], in0=rs_[:, 7:8], in1=rs_[:, 3:4], op=ALU.mult),
              reads=[K("rs7"), K("rs3")], writes=[K("rs8")])
            D(lambda e: e.tensor_tensor(out=rs_[:, 9:10], in0=rs_[:, 3:4], in1=rs_[:, 8:9], op=ALU.subtract),
              reads=[K("rs8"), K("rs3")], writes=[K("rs9")])
            for g in range(4):
                D(lambda e, g=g: e.tensor_scalar(out=ind32[:, 8 * g:8 * g + 8], in0=m12[:], scalar1=oh_g[:, g:g + 1],
                                                 scalar2=None, op0=ALU.mult),
                  reads=[K("m12"), K("oh_g")], writes=[("ind32", par, g)])
                D(lambda e, g=g: e.tensor_scalar(out=ind1[:, 8 * g:8 * g + 8], in0=m1[:], scalar1=oh_g[:, g:g + 1],
                                                 scalar2=None, op0=ALU.mult),
                  reads=[K("m1"), K("oh_g")], writes=[("ind1", par, g)])
            i32k = [("ind32", par, g) for g in range(4)]
            i1k = [("ind1", par, g) for g in range(4)]
            D(lambda e: e.tensor_tensor(out=ind2[:], in0=ind32[:], in1=ind1[:], op=ALU.subtract),
              reads=i32k + i1k, writes=[K("ind2")])

        def route_pos(L, ti, par, xmb, xmbk):
            route_pos_pe(L, ti, par)
            route_pos_rest(L, ti, par, xmb, xmbk)

        def route_pos_pe(L, ti, par):
            ind32 = RS[par]["ind32"]
            i32k = [("ind32", par, g) for g in range(4)]
            if ti == 0:
                P.pe(lambda e: e.matmul(R0[:, 64:96], lhsT=Umat[:], rhs=ind32[:], start=True, stop=True),
                     reads=["Umat"] + i32k, writes=["ps_R0"])
            else:
                P.pe(lambda e: e.matmul(R0[:, 64:96], lhsT=Umat[:], rhs=ind32[:], start=True, stop=False),
                     reads=["Umat"] + i32k, writes=["ps_R0"])
                P.pe(lambda e: e.matmul(R0[:, 64:96], lhsT=ONESf[:], rhs=indcum[:], start=False, stop=True),
                     reads=["ONESf", "indcum"], writes=["ps_R0"])

        def route_pos_rest(L, ti, par, xmb, xmbk):
            S = RS[par]
            rs_, ind32, ind1, ind2 = S["rs"], S["ind32"], S["ind1"], S["ind2"]
            idx1, idx2, wt1, wt2 = idx1a[:, L, :], idx2a[:, L, :], wt1a[:, L, :], wt2a[:, L, :]
            K = lambda n: (n, par)
            D = P.dve
            i32k = [("ind32", par, g) for g in range(4)]
            i1k = [("ind1", par, g) for g in range(4)]
            D(lambda e: e.tensor_tensor(out=slotv[:], in0=R0[:, 64:96], in1=ebase[:], op=ALU.add),
              reads=["ps_R0", "ebase"], writes=["slotv"])
            D(lambda e: e.tensor_scalar(out=valid[:], in0=R0[:, 64:96], scalar1=float(CAP), scalar2=None, op0=ALU.is_lt),
              reads=["ps_R0"], writes=["valid"])
            if ti == 0:
                D(lambda e: e.tensor_copy(out=indcum[:], in_=ind32[:]), reads=i32k, writes=["indcum"])
            else:
                D(lambda e: e.tensor_tensor(out=indcum[:], in0=indcum[:], in1=ind32[:], op=ALU.add),
                  reads=i32k + ["indcum"], writes=["indcum"])
            for (k, indk, indt, wcol, idxt, wtt) in [(0, i1k, ind1, 8, idx1, wt1), (1, [K("ind2")], ind2, 9, idx2, wt2)]:
                D(lambda e, indt=indt: e.tensor_tensor(out=tmp32[:], in0=indt[:], in1=slotv[:], op=ALU.mult),
                  reads=list(indk) + ["slotv"], writes=["tmp32"])
                D(lambda e, k=k: e.tensor_reduce(out=idxf[:, k:k + 1], in_=tmp32[:], axis=AX.X, op=ALU.add),
                  reads=["tmp32"], writes=[("idxf", k)])
                D(lambda e, indt=indt: e.tensor_tensor(out=tmp32[:], in0=indt[:], in1=valid[:], op=ALU.mult),
                  reads=list(indk) + ["valid", ("idxf", k)], writes=["tmp32"])
                D(lambda e, k=k: e.tensor_reduce(out=rs_[:, 10 + k:11 + k], in_=tmp32[:], axis=AX.X, op=ALU.add),
                  reads=["tmp32"], writes=[K(("ok", k))])
                D(lambda e, k=k, wcol=wcol, wtt=wtt: e.tensor_tensor(out=wtt[:, ti:ti + 1], in0=rs_[:, wcol:wcol + 1],
                                                                     in1=rs_[:, 10 + k:11 + k], op=ALU.mult),
                  reads=[K("rs8"), K("rs9"), K(("ok", k))], writes=[("wt", L, k, ti)])
                D(lambda e, k=k: e.tensor_scalar(out=rs_[:, 12 + k:13 + k], in0=rs_[:, 10 + k:11 + k], scalar1=-BIGIDX,
                                                 scalar2=BIGIDX, op0=ALU.mult, op1=ALU.add),
                  reads=[K(("ok", k))], writes=[K(("pen", k))])
                D(lambda e, k=k: e.tensor_tensor(out=idxf[:, k:k + 1], in0=idxf[:, k:k + 1], in1=rs_[:, 12 + k:13 + k],
                                                 op=ALU.add), reads=[("idxf", k), K(("pen", k))], writes=[("idxf", k)])
                D(lambda e, k=k, idxt=idxt: e.tensor_copy(out=idxt[:, ti:ti + 1], in_=idxf[:, k:k + 1]),
                  reads=[("idxf", k)], writes=[("idx", L, k, ti)])
            for (k, idxt) in [(0, idx1), (1, idx2)]:
                P.dma("gpsimd", lambda e, idxt=idxt: e.indirect_dma_start(
                    out=Xd, out_offset=bass.IndirectOffsetOnAxis(ap=idxt[:, ti:ti + 1], axis=0),
                    in_=xmb[:, :], in_offset=None, bounds_check=_bc(e), oob_is_err=False),
                    reads=[xmbk, ("idx", L, k, ti)] + ([("Xdz", r) for r in range(NSLOT // 256)] if (L == 0 and ti == 0) else []),
                    writes=[("Xd", L, ti, k)])

        def experts(L, ntiles):
            xd_keys = [("Xd", L, ti, k) for ti in range(ntiles) for k in range(2)]
            for E in range(NEXP):
                par = E % 2
                wg_v = ew(2 * par).rearrange("p (c f) -> p c f", f=512)
                wu_v = ew(2 * par + 1).rearrange("p (c f) -> p c f", f=512)
                wd_v = ew(4 + par).rearrange("p (c d) -> p c d", d=2048)
                kg, ku, kd = ("big", 2 * par), ("big", 2 * par + 1), ("big", 4 + par)
                P.dma("gpsimd", lambda e, E=E, wg_v=wg_v: e.dma_start(
                    out=wg_v, in_=wg_d[L, E].rearrange("(c p) f -> p c f", p=128)), writes=[kg])
                P.dma("gpsimd", lambda e, E=E, wu_v=wu_v: e.dma_start(
                    out=wu_v, in_=wu_d[L, E].rearrange("(c p) f -> p c f", p=128)), writes=[ku])
                P.dma("gpsimd", lambda e, E=E, wd_v=wd_v: e.dma_start(
                    out=wd_v, in_=wd_d[L, E].rearrange("(c p) d -> p c d", p=128)), writes=[kd])
                for sh in range(2):
                    P.dma("sync", lambda e, E=E, sh=sh: e.dma_start(
                        out=B4[sh][:], in_=Xd[E * CAP + sh * 128:E * CAP + (sh + 1) * 128, :]),
                        reads=xd_keys, writes=[("b4", sh)])
                for sh in range(2):
                    transpose_tile_bf16(lambda c, sh=sh: B4[sh][:, c * 128:(c + 1) * 128], [("b4", sh)],
                                        lambda c0, sh=sh: XET[:, c0:c0 + 8, sh * 128:(sh + 1) * 128], ["xet"])
                xetk = ["xet"]
                for (wv, wk, b0) in [(wg_v, kg, 0), (wu_v, ku, 2)]:
                    for fc in range(4):
                        bank = Y[b0 + fc // 2]
                        bk = K_Y[b0 + fc // 2]
                        for c in range(16):
                            P.pe(lambda e, wv=wv, fc=fc, c=c, bank=bank: e.matmul(
                                bank[:, (fc % 2) * 256:(fc % 2) * 256 + 256], lhsT=wv[:, c, fc * 128:(fc + 1) * 128],
                                rhs=XET[:, c, :], start=(c == 0), stop=(c == 15)),
                                reads=[wk] + xetk, writes=[bk])
                SG = G8[5]
                for b in range(2):
                    P.act(lambda e, b=b: e.activation(out=SG[:, b * 512:(b + 1) * 512], in_=Y[b][:], func=AF.Silu),
                          reads=[K_Y[b]], writes=[g8k(5)])
                    P.dve(lambda e, b=b: e.tensor_tensor(
                        out=HT[:, 2 * b:2 * b + 2, :], in0=SG[:, b * 512:(b + 1) * 512].rearrange("p (a b) -> p a b", b=256),
                        in1=Y[2 + b][:].rearrange("p (a b) -> p a b", b=256), op=ALU.mult),
                        reads=[g8k(5), K_Y[2 + b]], writes=[("ht", b)])
                htk = [("ht", 0), ("ht", 1)]
                for sh in range(2):
                    ye = G8[3 + sh]
                    for dc in range(4):
                        db = (sh * 4 + dc) % 2
                        bank, bk = (R0, "ps_R0") if db == 0 else (F0, "ps_F0")
                        for fc in range(4):
                            P.pe(lambda e, fc=fc, sh=sh, dc=dc, bank=bank, wd_v=wd_v: e.matmul(
                                bank[:], lhsT=HT[:, fc, sh * 128:(sh + 1) * 128], rhs=wd_v[:, fc, dc * 512:(dc + 1) * 512],
                                start=(fc == 0), stop=(fc == 3)), reads=htk + [kd], writes=[bk])
                        if dc % 2 == 0:
                            P.act(lambda e, ye=ye, dc=dc, bank=bank: e.copy(out=ye[:, dc * 512:(dc + 1) * 512], in_=bank[:]),
                                  reads=[bk], writes=[g8k(3 + sh)])
                        else:
                            P.dve(lambda e, ye=ye, dc=dc, bank=bank: e.tensor_copy(out=ye[:, dc * 512:(dc + 1) * 512], in_=bank[:]),
                                  reads=[bk], writes=[g8k(3 + sh)])
                    P.dma("sync", lambda e, E=E, sh=sh, ye=ye: e.dma_start(
                        out=Yd[E * CAP + sh * 128:E * CAP + (sh + 1) * 128, :], in_=ye[:]),
                        reads=[g8k(3 + sh)], writes=[("Yd", L, E)])

        def combine_tile(L, ti, ntot_exp, xm_src, gi, dest_fn):
            ydk = [("Yd", L, E) for E in range(ntot_exp)]
            idx1, idx2, wt1, wt2 = idx1a[:, L, :], idx2a[:, L, :], wt1a[:, L, :], wt2a[:, L, :]
            ya, yb = (0, 1) if ti % 2 == 0 else (4, 5)
            y1, y2, xm, z = G8[ya], G8[yb], G8[2], G8[3]
            for (k, idxt, yt, yk) in [(0, idx1, y1, g8k(ya)), (1, idx2, y2, g8k(yb))]:
                P.dma("gpsimd", lambda e, idxt=idxt, yt=yt: e.indirect_dma_start(
                    out=yt[:, :], out_offset=None, in_=Yd,
                    in_offset=bass.IndirectOffsetOnAxis(ap=idxt[:, ti:ti + 1], axis=0),
                    bounds_check=_bc(e), oob_is_err=False),
                    reads=ydk + [("idx", L, k, ti)], writes=[yk])
            P.dma("sync", lambda e: e.dma_start(out=xm[:], in_=xm_src), reads=[("XMs", L, ti)], writes=[g8k(2)])
            P.dve(lambda e: e.tensor_scalar(out=y1[:], in0=y1[:], scalar1=wt1[:, ti:ti + 1], scalar2=None, op0=ALU.mult),
                  reads=[g8k(ya), ("wt", L, 0, ti)], writes=[g8k(ya)])
            P.dve(lambda e: e.scalar_tensor_tensor(out=y1[:], in0=y2[:], scalar=wt2[:, ti:ti + 1], in1=y1[:],
                                                   op0=ALU.mult, op1=ALU.add),
                  reads=[g8k(ya), g8k(yb), ("wt", L, 1, ti)], writes=[g8k(ya)])
            P.dve(lambda e: e.scalar_tensor_tensor(out=z[:], in0=xm[:], scalar=ALPHA, in1=y1[:],
                                                   op0=ALU.mult, op1=ALU.add),
                  reads=[g8k(ya), g8k(2)], writes=[g8k(3)])
            layer_norm(z, g8k(3), gvecs[gi][0], gvecs[gi][1], gvecs[gi][2], xm, g8k(2))
            return dest_fn(xm, g8k(2))

        GV = [sb("GVg", [128, 2048], F32), sb("GVb", [128, 2048], F32)]
        gvecs = {}

        def load_gb(gi):
            P.dma("sync", lambda e: e.dma_start(out=GV[0][:], in_=lng_d[gi]), writes=["GVg"])
            P.dma("sync", lambda e: e.dma_start(out=GV[1][:], in_=lnb_d[gi]), writes=["GVb"])
            gvecs[gi] = (GV[0], GV[1], ("GVg", "GVb"))

        def load_router(L):
            P.dma("sync", lambda e: e.dma_start(out=wr_sb[:], in_=wr_d[L].rearrange("(c p) n -> p c n", p=128)),
                  writes=["wr"])
            P.dma("sync", lambda e: e.dma_start(out=br_sb[:], in_=br_d[L]), writes=["br"])

        P.dma("gpsimd", lambda e: e.dma_start(out=wbig, in_=wo_d.rearrange("(c p) n -> p c n", p=128)), writes=K_wbig)
        load_gb(0)
        load_router(0)
        xmT_v = G8[5][:].rearrange("p (c t) -> p c t", t=128)
        def b1_load(ti):
            ab = B4[ti % 2]
            xt = G8[ti % 2]
            P.dma("gpsimd", lambda e, ti=ti, ab=ab: e.dma_start(out=ab[:], in_=ain[ti * 128:(ti + 1) * 128, :]),
                  writes=[("b4", ti % 2)])
            P.dma("sync", lambda e, ti=ti, xt=xt: e.dma_start(out=xt[:], in_=xin[ti * 128:(ti + 1) * 128, :]),
                  writes=[g8k(ti % 2)])

        def b1_a1(ti):
            ab = B4[ti % 2]
            transpose_tile_bf16(lambda c, ab=ab: ab[:, c * 128:(c + 1) * 128], [("b4", ti % 2)],
                                lambda c0: T16[:, c0:c0 + 8, :], ["xet"], all_act=True)
            for dc in range(4):
                for c in range(16):
                    P.pe(lambda e, dc=dc, c=c: e.matmul(Y[dc][:], lhsT=T16[:, c, :], rhs=wbig[:, c, dc * 512:(dc + 1) * 512],
                                                        start=(c == 0), stop=(c == 15)),
                         reads=["xet"] + K_wbig, writes=[K_Y[dc]])

        def b1_z(ti):
            xt = G8[ti % 2]
            z = G8[2]
            for dc in range(4):
                P.dve(lambda e, dc=dc, xt=xt: e.scalar_tensor_tensor(
                    out=z[:, dc * 512:(dc + 1) * 512], in0=xt[:, dc * 512:(dc + 1) * 512], scalar=ALPHA, in1=Y[dc][:],
                    op0=ALU.mult, op1=ALU.add), reads=[g8k(ti % 2), K_Y[dc]], writes=[g8k(2)])

        def b1_ln(ti):
            xm = G8[3 + ti % 2]
            xmk = g8k(3 + ti % 2)
            layer_norm(G8[2], g8k(2), gvecs[0][0], gvecs[0][1], gvecs[0][2], xm, xmk)
            P.dma("sync", lambda e, ti=ti, xm=xm: e.dma_start(out=XM[0, ti * 128:(ti + 1) * 128, :], in_=xm[:]),
                  reads=[xmk], writes=[("XMs", 0, ti)])

        b1_load(0)
        b1_load(1)
        for j in range(NTL + 3):
            if 0 <= j - 3 < NTL:
                route_pos_pe(0, j - 3, (j - 3) % 2)
            if 0 <= j - 1 < NTL:
                b1_z(j - 1)
            if j < NTL:
                b1_a1(j)
            if 0 <= j - 1 < NTL:
                if j + 1 < NTL:
                    b1_load(j + 1)
                b1_ln(j - 1)
            if 0 <= j - 3 < NTL:
                t_ = j - 3
                route_pos_rest(0, t_, t_ % 2, B4[2 + t_ % 2], ("b4", 2 + t_ % 2))
            if 0 <= j - 2 < NTL:
                route_lg((j - 2) % 2)
            if 0 <= j - 1 < NTL:
                t_ = j - 1
                route_front(0, G8[3 + t_ % 2], g8k(3 + t_ % 2), t_, t_ % 2, xmT_v, g8k(5),
                            B4[2 + t_ % 2], ("b4", 2 + t_ % 2), do_lg=False)
            if 0 <= j - 2 < NTL:
                t_ = j - 2
                route_math(0, t_, t_ % 2)
        experts(0, NTL)
        load_gb(1)

        def dest_l0(ti):
            def f(xt, xk):
                return P.dma("sync", lambda e: e.dma_start(out=X1[ti * 128:(ti + 1) * 128, :], in_=xt[:]),
                             reads=[xk], writes=[("X1s", ti)])
            return f
        for ti in range(NTL):
            o = combine_tile(0, ti, NEXP, XM[0, ti * 128:(ti + 1) * 128, :], 1, dest_l0(ti))
            if not do_layer1 and ti >= 1:
                out_ops.append(P.dma("sync", lambda e, ti=ti: e.dma_start(out=out_d[(ti - 1) * 128:ti * 128, :], in_=G8[2][:]),
                                     reads=[g8k(2)]))
        if do_layer1:
            NCH = NMAIN // 2
            CW = 384
            X1T = BIG[:, 32768:32768 + 16 * CW].rearrange("p (c t) -> p c t", t=CW)
            XMT1 = BIG[:, 40960:45056].bitcast(F32).rearrange("p (c t) -> p c t", t=128)
            ALLK = [("big", 4), ("big", 5)]
            P.dve(lambda e: e.tensor_copy(out=rs[:, 15:16], in_=rs[:, 15:16]), reads=["rs15"] + ALLK,
                  writes=["rs15", "xmt1"] + [("dg", k) for k in range(31)] + ALLK + [("x1t", j) for j in range(3)] + [("st", cc) for cc in range(16)])
            P.dma("gpsimd", lambda e: e.dma_start(out=wbig, in_=wpw2_d.rearrange("(c p) n -> p c n", p=128)),
                  writes=K_wbig)
            load_gb(2)
            load_router(1)
            UT = [G8[4], G8[5]]

            def ut(cc):
                return UT[cc // 8][:, (cc % 8) * 256:(cc % 8 + 1) * 256]

            def utk(cc):
                return g8k(4 + cc // 8)
            ACC = sb("ACC", [128, 2, 256], F32)
            HTc = [sb(f"HTc{i}", [128, CW], BF16) for i in range(2)]
            SGc = sb("SGc", [128, CW], F32)
            DG = BIG[:, 45056:45056 + 31 * 128].rearrange("p (k c) -> p k c", c=128)
            USQ = [sb(f"USQ{i}", [128, 512], BF16) for i in range(2)]
            ONESb = sb("ONESb", [128, 128], BF16)
            P.dve(lambda e: e.tensor_copy(out=ONESb[:], in_=ONESf[:]), reads=["ONESf"], writes=["ONESb"])
            MR = sb("MR", [128, 2, 256], F32)
            wbufs = [
                (B4[2][:].rearrange("p (c n) -> p c n", n=128), ("b4", 2),
                 B4[3][:].rearrange("p (c n) -> p c n", n=128), ("b4", 3)),
                (XETf[:, 0:2048].rearrange("p (c n) -> p c n", n=128), "xet",
                 XETf[:, 2048:4096].rearrange("p (c n) -> p c n", n=128), "xet"),
            ]
            for q in range(NCH):
                x1tk = [("x1t", j) for j in range(3)]
                stk = [("st", cc) for cc in range(16)]
                P.dve(lambda e: e.tensor_copy(out=rs[:, 15:16], in_=rs[:, 15:16]), reads=["rs15"],
                      writes=["rs15"] + x1tk + stk)
                for j in range(3):
                    gt = 2 * q + j
                    ab = B4[j % 2]
                    P.dma("gpsimd", lambda e, gt=gt, ab=ab: e.dma_start(out=ab[:], in_=X1[gt * 128:(gt + 1) * 128, :]),
                          reads=[("X1s", gt)], writes=[("b4", j % 2)])
                    transpose_tile_bf16(lambda c, ab=ab: ab[:, c * 128:(c + 1) * 128], [("b4", j % 2)],
                                        lambda c0, j=j: X1T[:, c0:c0 + 8, j * 128:(j + 1) * 128],
                                        [("x1t", j)])
                def stage_ag(cc):
                    wch, wk, gch, gk = wbufs[cc % 2]
                    ba = (cc % 2) * 2
                    P.dma("gpsimd", lambda e, cc=cc, wch=wch: e.dma_start(
                        out=wch, in_=wpw1_d[:, cc * 128:(cc + 1) * 128].rearrange("(c p) n -> p c n", p=128)),
                        writes=[wk])
                    P.dma("gpsimd", lambda e, cc=cc, gch=gch: e.dma_start(
                        out=gch, in_=wpw1_d[:, 2048 + cc * 128:2048 + (cc + 1) * 128].rearrange("(c p) n -> p c n", p=128)),
                        writes=[gk])
                    for (wv, wkk, bi) in [(wch, wk, ba), (gch, gk, ba + 1)]:
                        for c in range(16):
                            P.pe(lambda e, wv=wv, c=c, bi=bi: e.matmul(
                                Y[bi][:, 0:CW], lhsT=wv[:, c, :], rhs=X1T[:, c, :],
                                start=(c == 0), stop=(c == 15)), reads=[wkk] + x1tk, writes=[K_Y[bi]])
                    hT = HTc[cc % 2]
                    hk = ("htc", cc % 2)
                    P.act(lambda e, cc=cc, ba=ba: e.activation(out=SGc[:], in_=Y[ba + 1][:, 0:CW], func=AF.Sigmoid,
                                                               bias=bpw1[:, 16 + cc:17 + cc], scale=1.0),
                          reads=[K_Y[ba + 1], "bpw1"], writes=["sgc"])
                    P.dve(lambda e, cc=cc, hT=hT, ba=ba: e.scalar_tensor_tensor(
                        out=hT[:], in0=Y[ba][:, 0:CW], scalar=bpw1[:, cc:cc + 1], in1=SGc[:],
                        op0=ALU.add, op1=ALU.mult), reads=[K_Y[ba], "sgc", "bpw1"], writes=[hk])
                    if q == 0:
                        P.dve(lambda e, hT=hT: e.tensor_scalar(out=hT[:, 0:128], in0=hT[:, 0:128], scalar1=halo[:, 0:1],
                                                               scalar2=None, op0=ALU.mult),
                              reads=[hk, "halo"], writes=[hk])

                def stage_dg(cc):
                    for k in range(31):
                        if k % 2 == 0:
                            P.act(lambda e, cc=cc, k=k: e.activation(out=DG[:, k, :], in_=ident[:], func=AF.Identity,
                                                                     scale=wdw[:, cc, k:k + 1]),
                                  reads=["ident", "wdw"], writes=[("dg", k)])
                        else:
                            P.dve(lambda e, cc=cc, k=k: e.tensor_scalar(out=DG[:, k, :], in0=ident[:],
                                                                        scalar1=wdw[:, cc, k:k + 1], scalar2=None,
                                                                        op0=ALU.mult),
                                  reads=["ident", "wdw"], writes=[("dg", k)])

                def stage_conv(cc):
                    hT = HTc[cc % 2]
                    hk = ("htc", cc % 2)
                    u = ut(cc)
                    for k in range(31):
                        P.pe(lambda e, k=k, hT=hT: e.matmul(
                            R0[:, 0:256], lhsT=DG[:, k, :], rhs=hT[:, 98 + k:98 + k + 256],
                            start=(k == 0), stop=(k == 30)), reads=[("dg", k), hk], writes=["ps_R0"])
                    P.act(lambda e, cc=cc, u=u: e.activation(out=u, in_=R0[:, 0:256], func=AF.Identity,
                                                             bias=bdw[:, cc:cc + 1], scale=1.0),
                          reads=["ps_R0", "bdw"], writes=[utk(cc)])
                    P.act(lambda e, u=u, cc=cc: e.activation(out=USQ[cc % 2][:, 256:512], in_=u, func=AF.Square),
                          reads=[utk(cc)], writes=[("usq", cc % 2)])
                    P.dve(lambda e, u=u, cc=cc: e.tensor_copy(out=USQ[cc % 2][:, 0:256], in_=u),
                          reads=[utk(cc)], writes=[("ub", cc % 2)])

                def stage_stats(cc):
                    u = ut(cc)
                    P.pe(lambda e, cc=cc: e.matmul(F0[:], lhsT=ONESb[:], rhs=USQ[cc % 2][:], start=True, stop=True),
                         reads=["ONESb", ("usq", cc % 2), ("ub", cc % 2)], writes=["ps_F0"])
                    if cc == 0:
                        P.dve(lambda e: e.tensor_copy(out=ACC[:].rearrange("p a b -> p (a b)"), in_=F0[:]),
                              reads=["ps_F0"], writes=["acc0", "acc1"])
                    else:
                        P.dve(lambda e: e.tensor_tensor(out=ACC[:].rearrange("p a b -> p (a b)"),
                                                        in0=ACC[:].rearrange("p a b -> p (a b)"), in1=F0[:], op=ALU.add),
                              reads=["ps_F0", "acc0", "acc1"], writes=["acc0", "acc1"])

                for i in range(18):
                    if 0 <= i - 1 < 16:
                        stage_dg(i - 1)
                    if i < 16:
                        stage_ag(i)
                    if 0 <= i - 1 < 16:
                        stage_conv(i - 1)
                    if 0 <= i - 2 < 16:
                        stage_stats(i - 2)
                P.dve(lambda e: e.tensor_scalar(out=MR[:, 0, :], in0=ACC[:, 0, :], scalar1=1.0 / 2048, scalar2=None,
                                                op0=ALU.mult), reads=["acc0"], writes=["mr0"])
                P.dve(lambda e: e.tensor_tensor(out=MR[:, 1, :], in0=MR[:, 0, :], in1=MR[:, 0, :], op=ALU.mult),
                      reads=["mr0"], writes=["mr1"])
                P.dve(lambda e: e.scalar_tensor_tensor(out=MR[:, 1, :], in0=ACC[:, 1, :], scalar=1.0 / 2048, in1=MR[:, 1, :],
                                                       op0=ALU.mult, op1=ALU.subtract),
                      reads=["acc1", "mr1"], writes=["mr1"])
                P.act(lambda e: e.activation(out=MR[:, 1, :], in_=MR[:, 1, :], func=AF.Ln, bias=epsv[:, 0:1], scale=1.0),
                      reads=["mr1", "epsv"], writes=["mr1"])
                P.act(lambda e: e.activation(out=MR[:, 1, :], in_=MR[:, 1, :], func=AF.Exp, scale=-0.5),
                      reads=["mr1"], writes=["mr1"])
                for cc in range(16):
                    u = ut(cc)
                    eng = P.dve
                    eng(lambda e, u=u: e.tensor_tensor(out=u, in0=u, in1=MR[:, 0, :], op=ALU.subtract),
                        reads=[utk(cc), "mr0"], writes=[utk(cc)])
                    eng(lambda e, u=u: e.tensor_tensor(out=u, in0=u, in1=MR[:, 1, :], op=ALU.mult),
                        reads=[utk(cc), "mr1"], writes=[utk(cc)])
                    P.act(lambda e, u=u, cc=cc: e.activation(out=X1T[:, cc, 0:256], in_=u, func=AF.Silu,
                                                            bias=cb[:, cc:cc + 1], scale=cg[:, cc:cc + 1]),
                          reads=[utk(cc), "cg", "cb"], writes=[("st", cc)] + x1tk)
                xm_bufs = [(G8[3], g8k(3)), (G8[4], g8k(4))]
                for t in range(2):
                    gt = 2 * q + 1 + t
                    l1t = gt - 1
                    xt = G8[t % 2]
                    P.dma("sync", lambda e, gt=gt, xt=xt: e.dma_start(out=xt[:], in_=X1[gt * 128:(gt + 1) * 128, :]),
                          reads=[("X1s", gt)], writes=[g8k(t % 2)])
                def tail_pw2(t):
                    for dc in range(4):
                        for cc in range(16):
                            P.pe(lambda e, dc=dc, cc=cc, t=t: e.matmul(
                                Y[dc][:], lhsT=X1T[:, cc, t * 128:(t + 1) * 128], rhs=wbig[:, cc, dc * 512:(dc + 1) * 512],
                                start=(cc == 0), stop=(cc == 15)), reads=stk + K_wbig, writes=[K_Y[dc]])

                def tail_z(t):
                    xt = G8[t % 2]
                    z = G8[2]
                    for dc in range(4):
                        P.dve(lambda e, dc=dc, xt=xt: e.scalar_tensor_tensor(
                            out=z[:, dc * 512:(dc + 1) * 512], in0=xt[:, dc * 512:(dc + 1) * 512], scalar=ALPHA,
                            in1=Y[dc][:], op0=ALU.mult, op1=ALU.add), reads=[g8k(t % 2), K_Y[dc]], writes=[g8k(2)])

                def tail_ln(t):
                    l1t = 2 * q + t
                    xm, xmk = xm_bufs[t]
                    layer_norm(G8[2], g8k(2), gvecs[2][0], gvecs[2][1], gvecs[2][2], xm, xmk)
                    P.dma("sync", lambda e, l1t=l1t, xm=xm: e.dma_start(out=XM[1, l1t * 128:(l1t + 1) * 128, :], in_=xm[:]),
                          reads=[xmk], writes=[("XMs", 1, l1t)])

                tail_pw2(0)
                tail_z(0)
                tail_pw2(1)
                tail_ln(0)
                tail_z(1)
                tail_ln(1)
                for t in range(2):
                    l1t = 2 * q + t
                    xm, xmk = xm_bufs[t]
                    route_front(1, xm, xmk, l1t, t, XMT1, "xmt1", B4[t % 2], ("b4", t % 2))
                    route_math(1, l1t, t)
                for t in range(2):
                    l1t = 2 * q + t
                    route_pos(1, l1t, t, B4[t % 2], ("b4", t % 2))
            P.dve(lambda e: e.tensor_copy(out=rs[:, 15:16], in_=rs[:, 15:16]), reads=["rs15"],
                  writes=["rs15", "xmt1"] + [("dg", k) for k in range(31)] + ALLK + [("x1t", j) for j in range(3)] + [("st", cc) for cc in range(16)])
            experts(1, NMAIN)
            load_gb(3)

            def dest_l1(ti):
                def f(xt, xk):
                    return P.dma("sync", lambda e: e.dma_start(out=out_d[ti * 128:(ti + 1) * 128, :], in_=xt[:]),
                                 reads=[xk])
                return f
            for ti in range(NMAIN):
                o = combine_tile(1, ti, NEXP, XM[1, ti * 128:(ti + 1) * 128, :], 3, dest_l1(ti))
                out_ops.append(o)
        P.finish(out_ops)
        stats = P.build(st)
        print("rest stats", stats)
    return nc


N_CORES = 8
SEQ = 16384


def _attn_core_inputs(core, x2d, wqkv):
    w = np.empty((2, 2048, 384), np.float32)
    for j in range(2):
        h = 2 * core + j
        w[j, :, 0:128] = wqkv[:, h * 128:(h + 1) * 128]
        w[j, :, 128:256] = wqkv[:, 2048 + h * 128:2048 + (h + 1) * 128]
        w[j, :, 256:384] = wqkv[:, 4096 + h * 128:4096 + (h + 1) * 128]
    bt, fc = attn_consts(core)
    return {"x": x2d, "wqkv": w, "btab": bt, "fcorr": fc}


def _rest_core_inputs(d, core, x_full, attn_o, NTL):
    T = NTL - 1
    t0 = core * T * 128

    def halo_slice(a):
        out = np.zeros((NTL * 128, a.shape[1]), np.float32)
        if t0 >= 128:
            out[:] = a[t0 - 128:t0 + T * 128]
        else:
            out[128:] = a[t0:t0 + T * 128]
        return out

    def rep(v):
        return np.ascontiguousarray(np.broadcast_to(v[None, :], (128, v.shape[0]))).astype(np.float32)

    def fm(v, n):
        return np.ascontiguousarray(v.reshape(n, 128).T)

    inp = {}
    inp["xin"] = halo_slice(x_full)
    inp["ain"] = halo_slice(attn_o)
    inp["wo"] = d["attn_w_o"][0]
    inp["lng"] = np.stack([rep(d["mix_ln_g"][0]), rep(d["ffn_ln_g"][0]), rep(d["mix_ln_g"][1]), rep(d["ffn_ln_g"][1])])
    inp["lnb"] = np.stack([rep(d["mix_ln_b"][0]), rep(d["ffn_ln_b"][0]), rep(d["mix_ln_b"][1]), rep(d["ffn_ln_b"][1])])
    inp["wr"] = np.ascontiguousarray(np.concatenate([d["moe_w_grp"], d["moe_w_rt"].reshape(2, 2048, 32)], axis=2))
    br = np.concatenate([d["moe_b_grp"], d["moe_b_rt"].reshape(2, 32)], axis=1)
    inp["br"] = np.stack([rep(br[0]), rep(br[1])])
    inp["wgate"] = d["moe_w_gate"].reshape(2, 32, 2048, 512)
    inp["wup"] = d["moe_w_up"].reshape(2, 32, 2048, 512)
    inp["wdown"] = d["moe_w_down"].reshape(2, 32, 512, 2048)
    inp["wpw1"] = d["conv_w_pw1"][0]
    inp["bpw1"] = fm(d["conv_b_pw1"][0], 32)
    inp["wdw"] = np.ascontiguousarray(d["conv_w_dw"][0].reshape(31, 16, 128).transpose(2, 1, 0))
    inp["bdw"] = fm(d["conv_b_dw"][0], 16)
    inp["cg"] = fm(d["conv_ln_g"][0], 16)
    inp["cb"] = fm(d["conv_ln_b"][0], 16)
    inp["wpw2"] = d["conv_w_pw2"][0]
    inp["halo"] = np.full((128, 1), 0.0 if core == 0 else 1.0, np.float32)
    inp["ebase"] = rep(np.arange(32, dtype=np.float32) * CAP)
    return inp


def kernel(**inputs):
    d = {k: np.asarray(v, dtype=np.float32) for k, v in inputs.items()}
    x2d = np.ascontiguousarray(d["x"][0])
    wqkv = d["attn_w_qkv"][0]
    nc_a = build_attn(SEQ // 512)
    res_a = run_bass_kernel_spmd(nc_a, [_attn_core_inputs(c, x2d, wqkv) for c in range(N_CORES)],
                                 core_ids=list(range(N_CORES)))
    attn_o = np.concatenate([r["o"] for r in res_a.results], axis=1)
    NTL = SEQ // N_CORES // 128 + 1
    nc_b = build_rest(NTL, 32, True)
    res_b = run_bass_kernel_spmd(nc_b, [_rest_core_inputs(d, c, x2d, attn_o, NTL) for c in range(N_CORES)],
                                 core_ids=list(range(N_CORES)))
    out = np.concatenate([r["out"] for r in res_b.results], axis=0)
    return out.reshape(1, SEQ, 2048).astype(np.float32)
```
